# Optimizing a Trainium2 kernel written in Bass

```python
import math
import jax, jax.numpy as jnp
from jax import lax
import numpy as np

D_MODEL = 1024
BATCH = 4
SEQ = 8192
DEPTH = 1

GRID_W = 64
HEAD_DIM = 64
A_HEADS = D_MODEL // (2 * HEAD_DIM)
A_KV_HEADS = A_HEADS // 4
B_HEADS = D_MODEL // (2 * HEAD_DIM)
A_WIDTH = A_HEADS * HEAD_DIM
A_KV_WIDTH = A_KV_HEADS * HEAD_DIM
B_WIDTH = B_HEADS * HEAD_DIM
MIX_WIDTH = A_WIDTH + B_WIDTH
IN_WIDTH = A_WIDTH + 2 * A_KV_WIDTH + 3 * B_WIDTH
Q_BLOCK = 128
ROPE_THETA = 10000.0
NA_KH = 8
NA_KW = 16
PEER_HEADS = 8
PEER_N_KEYS = 128
PEER_N_EXPERTS = PEER_N_KEYS * PEER_N_KEYS
PEER_QUERY_DIM = 256
PEER_SUB_DIM = PEER_QUERY_DIM // 2
PEER_TOPK = 16
PEER_BLOCK = 128
EPS = 1e-6

kernel_name = "hymba_gqa_natten_peer_block"


def rmsnorm(x, g):
    xf = x.astype(jnp.float32)
    y = xf * lax.rsqrt(jnp.mean(xf * xf, axis=-1, keepdims=True) + EPS)
    return (y * g.astype(jnp.float32)).astype(x.dtype)


def rope_axis(x, pos):
    half = x.shape[-1] // 2
    freqs = ROPE_THETA ** (-jnp.arange(half, dtype=jnp.float32) / half)
    ang = pos.astype(jnp.float32)[:, None] * freqs[None, :]
    cos = jnp.cos(ang)[None, :, None, :]
    sin = jnp.sin(ang)[None, :, None, :]
    xf = x.astype(jnp.float32)
    x1, x2 = xf[..., :half], xf[..., half:]
    out = jnp.concatenate([x1 * cos - x2 * sin, x1 * sin + x2 * cos], axis=-1)
    return out.astype(x.dtype)


def axial_rope(x, row, col):
    d = x.shape[-1] // 2
    return jnp.concatenate([rope_axis(x[..., :d], row), rope_axis(x[..., d:], col)], axis=-1)


def gqa_axial_attention(q, k, v, row, col, q_norm_g, k_norm_g):
    bsz, seq, nh, dh = q.shape
    grp = nh // A_KV_HEADS
    q = axial_rope(rmsnorm(q, q_norm_g), row, col)
    k = axial_rope(rmsnorm(k, k_norm_g), row, col)
    nblk = seq // Q_BLOCK
    qb = q.reshape(bsz, nblk, Q_BLOCK, A_KV_HEADS, grp, dh).transpose(1, 0, 2, 3, 4, 5)
    scale = dh ** -0.5

    def block(qblk):
        s = jnp.einsum('bqkgd,bskd->bkgqs', qblk, k).astype(jnp.float32) * scale
        p = jax.nn.softmax(s, axis=-1).astype(v.dtype)
        return jnp.einsum('bkgqs,bskd->bqkgd', p, v)

    o = lax.map(block, qb)
    return o.transpose(1, 0, 2, 3, 4, 5).reshape(bsz, seq, nh * dh)


def natten_tables(seq):
    rows = seq // GRID_W
    kh = min(NA_KH, rows)
    t = jnp.arange(seq, dtype=jnp.int32)
    r = t // GRID_W
    cc = t % GRID_W
    rs = jnp.clip(r - kh // 2, 0, rows - kh)
    cs = jnp.clip(cc - NA_KW // 2, 0, GRID_W - NA_KW)
    key_r = rs[:, None, None] + jnp.arange(kh, dtype=jnp.int32)[None, :, None]
    key_c = cs[:, None, None] + jnp.arange(NA_KW, dtype=jnp.int32)[None, None, :]
    shape = (seq, kh, NA_KW)
    idx = jnp.broadcast_to(key_r * GRID_W + key_c, shape).reshape(seq, kh * NA_KW)
    dri = jnp.broadcast_to(key_r - r[:, None, None] + (NA_KH - 1), shape).reshape(seq, kh * NA_KW)
    dci = jnp.broadcast_to(key_c - cc[:, None, None] + (NA_KW - 1), shape).reshape(seq, kh * NA_KW)
    return idx, dri, dci


def neighbourhood_attention(q, k, v, rpb):
    bsz, seq, nh, dh = q.shape
    idx, dri, dci = natten_tables(seq)
    nkeys = idx.shape[-1]
    nblk = seq // Q_BLOCK
    qb = q.reshape(bsz, nblk, Q_BLOCK, nh, dh).swapaxes(0, 1)
    idxb = idx.reshape(nblk, Q_BLOCK, nkeys)
    drb = dri.reshape(nblk, Q_BLOCK, nkeys)
    dcb = dci.reshape(nblk, Q_BLOCK, nkeys)
    scale = dh ** -0.5

    def block(args):
        qblk, ib, rb, cb = args
        kn = k[:, ib]
        vn = v[:, ib]
        bias = rpb[:, rb, cb].astype(jnp.float32)
        s = jnp.einsum('bqhd,bqlhd->bhql', qblk, kn).astype(jnp.float32) * scale + bias[None]
        p = jax.nn.softmax(s, axis=-1).astype(v.dtype)
        return jnp.einsum('bhql,bqlhd->bqhd', p, vn)

    o = lax.map(block, (qb, idxb, drb, dcb))
    return o.swapaxes(0, 1).reshape(bsz, seq, nh * dh)


def peer_ffn(h, w_query, sub_keys_1, sub_keys_2, peer_u, peer_v):
    bsz, seq, d = h.shape
    nblk = seq // PEER_BLOCK
    hb = h.reshape(bsz, nblk, PEER_BLOCK, d).swapaxes(0, 1)

    def block(hblk):
        q = (hblk @ w_query).reshape(bsz, PEER_BLOCK, PEER_HEADS, 2, PEER_SUB_DIM)
        s1 = jnp.einsum('bthd,nd->bthn', q[..., 0, :], sub_keys_1).astype(jnp.float32)
        s2 = jnp.einsum('bthd,nd->bthn', q[..., 1, :], sub_keys_2).astype(jnp.float32)
        v1, i1 = lax.top_k(s1, PEER_TOPK)
        v2, i2 = lax.top_k(s2, PEER_TOPK)
        cand = (v1[..., :, None] + v2[..., None, :]).reshape(bsz, PEER_BLOCK, PEER_HEADS, PEER_TOPK * PEER_TOPK)
        cidx = (i1[..., :, None] * PEER_N_KEYS + i2[..., None, :]).reshape(bsz, PEER_BLOCK, PEER_HEADS, PEER_TOPK * PEER_TOPK)
        top, pos = lax.top_k(cand, PEER_TOPK)
        eidx = jnp.take_along_axis(cidx, pos, axis=-1)
        g = jax.nn.softmax(top, axis=-1).astype(h.dtype)
        u = peer_u[eidx]
        a = jax.nn.gelu(jnp.einsum('bthkd,btd->bthk', u, hblk), approximate=False)
        vv = peer_v[eidx]
        return jnp.einsum('bthk,bthkd->btd', g * a, vv)

    o = lax.map(block, hb)
    return o.swapaxes(0, 1).reshape(bsz, seq, d)


def setup_inputs(seed: int = 0) -> dict:
    key = jax.random.key(seed)
    ks = jax.random.split(key, 20)
    f32 = jnp.float32
    nrm = lambda k, shape, s: jax.random.normal(k, shape, f32) * s
    gain = lambda k, shape: jnp.ones(shape, f32) + 0.01 * jax.random.normal(k, shape, f32)
    return {
        "x": nrm(ks[0], (BATCH, SEQ, D_MODEL), 1.0),
        "c": nrm(ks[1], (BATCH, D_MODEL), 1.0),
        "w_ada": nrm(ks[2], (DEPTH, D_MODEL, 6 * D_MODEL), D_MODEL ** -0.5),
        "b_ada": nrm(ks[3], (DEPTH, 6 * D_MODEL), 0.01),
        "norm1_g": gain(ks[4], (DEPTH, D_MODEL)),
        "w_in": nrm(ks[5], (DEPTH, D_MODEL, IN_WIDTH), D_MODEL ** -0.5),
        "q_norm_g": gain(ks[6], (DEPTH, HEAD_DIM)),
        "k_norm_g": gain(ks[7], (DEPTH, HEAD_DIM)),
        "natten_rpb": nrm(ks[8], (DEPTH, B_HEADS, 2 * NA_KH - 1, 2 * NA_KW - 1), 0.1),
        "group_norm_a_g": gain(ks[9], (DEPTH, A_WIDTH)),
        "group_norm_b_g": gain(ks[10], (DEPTH, B_WIDTH)),
        "w_out": nrm(ks[11], (DEPTH, MIX_WIDTH, D_MODEL), MIX_WIDTH ** -0.5),
        "norm2_g": gain(ks[12], (DEPTH, D_MODEL)),
        "peer_w_query": nrm(ks[13], (DEPTH, D_MODEL, PEER_HEADS * PEER_QUERY_DIM), D_MODEL ** -0.5),
        "peer_sub_keys_1": nrm(ks[14], (DEPTH, PEER_N_KEYS, PEER_SUB_DIM), PEER_SUB_DIM ** -0.5),
        "peer_sub_keys_2": nrm(ks[15], (DEPTH, PEER_N_KEYS, PEER_SUB_DIM), PEER_SUB_DIM ** -0.5),
        "peer_u": nrm(ks[16], (DEPTH, PEER_N_EXPERTS, D_MODEL), D_MODEL ** -0.5),
        "peer_v": nrm(ks[17], (DEPTH, PEER_N_EXPERTS, D_MODEL), PEER_HEADS ** -0.5),
        "final_norm_g": gain(ks[18], (D_MODEL,)),
    }


def reference(x, c, w_ada, b_ada, norm1_g, w_in, q_norm_g, k_norm_g, natten_rpb,
              group_norm_a_g, group_norm_b_g, w_out, norm2_g, peer_w_query,
              peer_sub_keys_1, peer_sub_keys_2, peer_u, peer_v, final_norm_g):
    bsz, seq, d = x.shape
    t = jnp.arange(seq, dtype=jnp.int32)
    row = t // GRID_W
    col = t % GRID_W
    o1 = A_WIDTH
    o2 = o1 + A_KV_WIDTH
    o3 = o2 + A_KV_WIDTH
    o4 = o3 + B_WIDTH
    o5 = o4 + B_WIDTH
    for l in range(DEPTH):
        mod = jax.nn.silu(c) @ w_ada[l] + b_ada[l]
        shift1, scale1, gate1, shift2, scale2, gate2 = jnp.split(mod, 6, axis=-1)
        h = rmsnorm(x, norm1_g[l]) * (1.0 + scale1[:, None, :]) + shift1[:, None, :]
        p = h @ w_in[l]
        qa = p[..., :o1].reshape(bsz, seq, A_HEADS, HEAD_DIM)
        ka = p[..., o1:o2].reshape(bsz, seq, A_KV_HEADS, HEAD_DIM)
        va = p[..., o2:o3].reshape(bsz, seq, A_KV_HEADS, HEAD_DIM)
        qb = p[..., o3:o4].reshape(bsz, seq, B_HEADS, HEAD_DIM)
        kb = p[..., o4:o5].reshape(bsz, seq, B_HEADS, HEAD_DIM)
        vb = p[..., o5:].reshape(bsz, seq, B_HEADS, HEAD_DIM)
        out_a = gqa_axial_attention(qa, ka, va, row, col, q_norm_g[l], k_norm_g[l])
        out_b = neighbourhood_attention(qb, kb, vb, natten_rpb[l])
        mix = jnp.concatenate([rmsnorm(out_a, group_norm_a_g[l]),
                               rmsnorm(out_b, group_norm_b_g[l])], axis=-1) @ w_out[l]
        x = x + gate1[:, None, :] * mix
        h2 = rmsnorm(x, norm2_g[l]) * (1.0 + scale2[:, None, :]) + shift2[:, None, :]
        ffn = peer_ffn(h2, peer_w_query[l], peer_sub_keys_1[l], peer_sub_keys_2[l], peer_u[l], peer_v[l])
        x = x + gate2[:, None, :] * ffn
    return rmsnorm(x, final_norm_g)
```

```python
import numpy as np
import ml_dtypes
import concourse.bass as bass
import concourse.mybir as mybir
from concourse.bass_utils import run_bass_kernel_spmd

F32 = mybir.dt.float32
BF16 = mybir.dt.bfloat16
I32 = mybir.dt.int32
U32 = mybir.dt.uint32
AF = mybir.ActivationFunctionType
ALU = mybir.AluOpType
AX = mybir.AxisListType


class Ctx:
    def __init__(self, nc):
        self.nc = nc
        self.eng = {'pe': nc.tensor, 'dve': nc.vector, 'act': nc.scalar, 'pool': nc.gpsimd, 'sp': nc.sync}
        self.sem = {k: nc.alloc_semaphore("s_" + k) for k in self.eng}
        self.cnt = {k: 0 for k in self.eng}
        self.know = {k: {} for k in self.eng}
        self.clock = {}
        self.last_w = {}
        self.readers = {}
        self.chan = {}
        self.names = {}
        self.n_wait = 0
        self.n_ins = 0
        self.out_events = []

    def sb(self, name, shape, dt):
        t = self.nc.alloc_sbuf_tensor(name, list(shape), dt)
        self.names[id(t)] = name
        return t

    def ps(self, name, shape, dt=F32):
        t = self.nc.alloc_psum_tensor(name, list(shape), dt)
        self.names[id(t)] = name
        return t

    def key(self, k):
        if isinstance(k, str):
            return k
        return self.names[id(k)]

    def _semof(self, src):
        if src in self.sem:
            return self.sem[src]
        return self.chan[src][0]

    def _need(self, e, ev, needs):
        if ev is None:
            return
        src, val = ev
        if src == 'pe' and e == 'pe':
            return
        if self.know[e].get(src, 0) >= val:
            return
        if needs.get(src, 0) < val:
            needs[src] = val

    def _collect(self, e, r, w):
        needs = {}
        for k in r:
            self._need(e, self.last_w.get(self.key(k)), needs)
        for k in w:
            kk = self.key(k)
            self._need(e, self.last_w.get(kk), needs)
            for src, val in self.readers.get(kk, {}).items():
                self._need(e, (src, val), needs)
        items = list(needs.items())
        for src, val in items:
            ck = self.clock.get((src, val), {})
            for s2, v2 in list(needs.items()):
                if s2 != src and ck.get(s2, 0) >= v2:
                    del needs[s2]
        for src, val in needs.items():
            self.eng[e].wait_ge(self._semof(src), val)
            self.n_wait += 1
            kn = self.know[e]
            if kn.get(src, 0) < val:
                kn[src] = val
            for s2, v2 in self.clock.get((src, val), {}).items():
                if kn.get(s2, 0) < v2:
                    kn[s2] = v2

    def _record(self, ev, r, w):
        for k in r:
            kk = self.key(k)
            d = self.readers.setdefault(kk, {})
            if d.get(ev[0], 0) < ev[1]:
                d[ev[0]] = ev[1]
        for k in w:
            kk = self.key(k)
            self.last_w[kk] = ev
            self.readers[kk] = {}

    cut = None

    def op(self, e, fn, r=(), w=(), dma=False, ch=None):
        if self.cut is not None and self.n_ins >= self.cut:
            return None
        if dma:
            return self._dma(e, fn, r, w, ch)
        self._collect(e, r, w)
        ins = fn(self.eng[e])
        self.cnt[e] += 1
        ins.then_inc(self.sem[e], 1)
        ev = (e, self.cnt[e])
        ck = dict(self.know[e])
        self.clock[ev] = ck
        self._record(ev, r, w)
        self.n_ins += 1
        return ev

    def _dma(self, q, fn, r, w, ch=None):
        self._collect(q, r, w)
        if ch is None:
            ch = self.key(w[0]) if len(w) else self.key(r[0])
        ch = "dma_" + ch + ("_sw" if q == 'pool' else "")
        if ch not in self.chan:
            self.chan[ch] = [self.nc.alloc_semaphore(ch), 0]
        ins = fn(self.eng[q])
        self.chan[ch][1] += 16
        ins.then_inc(self.chan[ch][0], 16)
        ev = (ch, self.chan[ch][1])
        self.clock[ev] = dict(self.know[q])
        self._record(ev, r, w)
        self.n_ins += 1
        return ev

    def dma(self, q, out, in_, r=(), w=(), ch=None, out_final=False):
        ev = self._dma(q, lambda e: e.dma_start(out=out, in_=in_), r, w, ch)
        if out_final:
            self.out_events.append(ev)
        return ev

    def finish(self):
        needs = {}
        for ev in self.out_events:
            if needs.get(ev[0], 0) < ev[1]:
                needs[ev[0]] = ev[1]
        for ch, (sem, count) in self.chan.items():
            if count > 0 and needs.get(ch, 0) < count:
                needs[ch] = count
        for src, val in needs.items():
            self.eng['sp'].wait_ge(self._semof(src), val)
        for e in ('pe', 'dve', 'act', 'pool'):
            if self.cnt[e] > 0:
                self.eng['sp'].wait_ge(self.sem[e], self.cnt[e])


def make_ident(c, identf, identb=None):
    n = 128
    nc = c.nc
    col = c.sb("mk_col", [n, n], F32)
    row = c.sb("mk_row", [n, 1], F32)
    c.op('pool', lambda e: e.iota(col[:], pattern=[[1, n]], base=0, channel_multiplier=0,
                                  allow_small_or_imprecise_dtypes=True), w=[col])
    c.op('pool', lambda e: e.iota(row[:], pattern=[[0, 1]], base=0, channel_multiplier=1,
                                  allow_small_or_imprecise_dtypes=True), w=[row])
    c.op('dve', lambda e: e.tensor_scalar(identf[:], col[:], row[:, 0:1], None, ALU.is_equal), r=[col, row], w=[identf])
    if identb is not None:
        c.op('dve', lambda e: e.tensor_copy(identb[:], identf[:]), r=[identf], w=[identb])


def _barrier(c):
    for e in ('pe', 'dve', 'act', 'pool', 'sp'):
        kn = c.know[e]
        for src in ('pe', 'dve', 'act', 'pool'):
            if c.cnt[src] > 0 and kn.get(src, 0) < c.cnt[src]:
                c.eng[e].wait_ge(c.sem[src], c.cnt[src])
                kn[src] = c.cnt[src]
        for ch, (sem, count) in c.chan.items():
            if count > 0 and kn.get(ch, 0) < count:
                c.eng[e].wait_ge(sem, count)
                kn[ch] = count


Ctx.barrier = _barrier

D_MODEL = 1024
NBLK_ALL = 64
NBLK_KVB = 34
EPS = 1e-6
NEG = -30000.0


class _Scope:
    def __init__(self, c, prefix):
        import contextlib
        self.c = c
        self.prefix = prefix
        self.stack = contextlib.ExitStack()

    def sb(self, name, shape, dt):
        t = self.stack.enter_context(self.c.nc.sbuf_tensor(self.prefix + name, list(shape), dt))
        self.c.names[id(t)] = self.prefix + name
        return t

    def ps(self, name, shape, dt=F32):
        t = self.stack.enter_context(self.c.nc.psum_tensor(self.prefix + name, list(shape), dt))
        self.c.names[id(t)] = self.prefix + name
        return t

    def close(self):
        self.stack.close()


def _bc(ap, axis, shape):
    return ap.unsqueeze(axis).to_broadcast(list(shape))


def build_nc(nown=32, nkv=NBLK_ALL, stop_after=None, dbg=0):
    nc = bass.Bass("TRN2", target_bir_lowering=False)
    DI = lambda name, shape, dt=F32: nc.dram_tensor(name, list(shape), dt, kind="ExternalInput").ap()
    xs = DI("xs", [8192, 1024])
    cT_d = DI("cT", [128, 8])
    w_ada = DI("w_ada", [1024, 6144])
    b_ada = DI("b_ada", [1, 6144])
    g1T_d = DI("g1T", [128, 8])
    g2row = DI("g2row", [1, 1024])
    gfrow = DI("gfrow", [1, 1024])
    w_in = DI("w_in", [1024, 2304])
    qg_d = DI("qg", [1, 64])
    kg_d = DI("kg", [1, 64])
    gnT_d = DI("gnT", [128, 8])
    w_out = DI("w_out", [1024, 1024])
    w_q = DI("w_q", [1024, 2048])
    k1T_d = DI("k1T", [128, 128])
    k2T_d = DI("k2T", [128, 128])
    peer_uv = DI("peer_uv", [16384, 2048])
    uvb = nc.dram_tensor("uvb_scr", [16384, 2048], BF16).ap()
    rope_d = DI("rope", [NBLK_ALL, 128, 2, 64])
    nbias_d = DI("nbias", [3, 128, 8, 640])
    out = nc.dram_tensor("out", [4096, 1024], F32, kind="ExternalOutput").ap()
    kbt_dram = nc.dram_tensor("kbt_scr", [NBLK_KVB, 128, 4, 128], BF16).ap()
    vb_dram = nc.dram_tensor("vb_scr", [NBLK_KVB, 128, 512], BF16).ap()

    c = Ctx(nc)
    op = c.op

    identf = c.sb("identf", [128, 128], F32)
    identb = c.sb("identb", [128, 128], BF16)
    make_ident(c, identf, identb)
    neghalf = c.sb("neghalf", [128, 8], F32)
    op('dve', lambda e: e.memset(neghalf[:], -0.5), w=[neghalf])
    iota16 = c.sb("iota16", [128, 16], F32)
    op('pool', lambda e: e.iota(iota16[:], pattern=[[1, 16]], base=0, channel_multiplier=0,
                                allow_small_or_imprecise_dtypes=True), w=[iota16])
    gate1_bc = c.sb("gate1_bc", [128, 1024], F32)
    gate2_bc = c.sb("gate2_bc", [128, 1024], F32)
    A2_bc = c.sb("A2_bc", [128, 1024], F32)
    B2_bc = c.sb("B2_bc", [128, 1024], F32)
    gf_bc = c.sb("gf_bc", [128, 1024], F32)
    modT = c.sb("modT", [128, 2, 8], F32)
    A1 = c.sb("A1", [128, 8], F32)
    g1T = c.sb("g1T_t", [128, 8], F32)
    gnT = c.sb("gnT_t", [128, 8], F32)
    qg = c.sb("qg_t", [128, 64], F32)
    kg = c.sb("kg_t", [128, 64], F32)
    negC = c.sb("negC", [128, 1], F32)
    k1T = c.sb("k1T_t", [128, 128], BF16)
    k2T = c.sb("k2T_t", [128, 128], BF16)

    c.dma('sp', g1T[:], g1T_d, w=[g1T])
    c.dma('sp', gnT[:], gnT_d, w=[gnT])
    c.dma('sp', qg[:], qg_d.to_broadcast([128, 64]), w=[qg])
    c.dma('sp', kg[:], kg_d.to_broadcast([128, 64]), w=[kg])
    c.dma('sp', gf_bc[:], gfrow.to_broadcast([128, 1024]), w=[gf_bc])
    c.dma('sp', A2_bc[:], g2row.to_broadcast([128, 1024]), w=[A2_bc])
    op('pool', lambda e: e.dma_start(out=k1T[:], in_=k1T_d), w=[k1T], dma=True)
    op('pool', lambda e: e.dma_start(out=k2T[:], in_=k2T_d), w=[k2T], dma=True)

    mq = c.sb("mq", [128, 2], F32)
    op('dve', lambda e: e.tensor_reduce(mq[:, 0:1], qg[:], AX.X, ALU.max, apply_absolute_value=True), r=[qg], w=[mq])
    op('dve', lambda e: e.tensor_reduce(mq[:, 1:2], kg[:], AX.X, ALU.max, apply_absolute_value=True), r=[kg, mq], w=[mq])
    op('dve', lambda e: e.scalar_tensor_tensor(negC[:], mq[:, 0:1], -8.0, mq[:, 1:2], ALU.mult, ALU.mult), r=[mq], w=[negC])

    S0 = _Scope(c, "s0_")
    cT = S0.sb("cT", [128, 8], F32)
    sc = S0.sb("sc", [128, 8], F32)
    scbc = S0.sb("scbc", [128, 8, 128], F32)
    wada = [S0.sb("wada%d" % i, [128, 8, 512], F32) for i in range(2)]
    bb = [S0.sb("bb%d" % i, [128, 512], F32) for i in range(2)]
    modg = [S0.sb("modg%d" % i, [128, 512], F32) for i in range(2)]
    psm = [S0.ps("psm%d" % i, [128, 512], F32) for i in range(2)]
    pst = S0.ps("pst", [128, 4, 128], F32)
    c.dma('sp', cT[:], cT_d, w=[cT])
    op('act', lambda e: e.activation(sc[:], cT[:], AF.Silu), r=[cT], w=[sc])
    op('dve', lambda e: e.tensor_copy(scbc[:], _bc(sc[:], 2, [128, 8, 128])), r=[sc], w=[scbc])
    w_ada_v = w_ada.rearrange("(j p) n -> p j n", p=128)
    for gi in range(12):
        wt = wada[gi % 2]; bt = bb[gi % 2]; mg = modg[gi % 2]; pm = psm[gi % 2]
        c.dma('sp', wt[:], w_ada_v[:, :, gi * 512:(gi + 1) * 512], w=[wt])
        c.dma('sp', bt[:], b_ada[0:1, gi * 512:(gi + 1) * 512].to_broadcast([128, 512]), w=[bt])
        for j in range(8):
            op('pe', lambda e: e.matmul(pm[:], scbc[:, j, :], wt[:, j, :], start=(j == 0), stop=(j == 7)),
               r=[scbc, wt], w=[pm])
        piece, half = gi // 2, gi % 2
        hs = slice(half * 512, (half + 1) * 512)
        if piece in (0, 1):
            op('dve', lambda e: e.tensor_tensor(mg[:], pm[:], bt[:], ALU.add), r=[pm, bt], w=[mg])
            for k in range(4):
                op('pe', lambda e: e.transpose(pst[:, k, :], mg[:, k * 128:(k + 1) * 128], identf[:]),
                   r=[mg, identf], w=[pst])
            op('dve', lambda e: e.tensor_copy(modT[:, piece, half * 4:(half + 1) * 4], pst[:, :, 0]), r=[pst], w=[modT])
        else:
            dst = {2: gate1_bc, 3: B2_bc, 4: None, 5: gate2_bc}[piece]
            if dst is not None:
                op('dve', lambda e: e.tensor_tensor(dst[:, hs], pm[:], bt[:], ALU.add), r=[pm, bt], w=[dst])
            else:
                op('dve', lambda e: e.tensor_tensor(mg[:], pm[:], bt[:], ALU.add), r=[pm, bt], w=[mg])
                op('dve', lambda e: e.scalar_tensor_tensor(A2_bc[:, hs], mg[:], 1.0, A2_bc[:, hs], ALU.add, ALU.mult),
                   r=[mg, A2_bc], w=[A2_bc])
    op('dve', lambda e: e.scalar_tensor_tensor(A1[:], modT[:, 1, :], 1.0, g1T[:], ALU.add, ALU.mult), r=[modT, g1T], w=[A1])
    c.barrier()
    S0.close()
    if stop_after == 'S0':
        c.dma('sp', out[0:128, :], gate1_bc[:], r=[gate1_bc], out_final=True)
        c.dma('sp', out[128:256, :], A2_bc[:], r=[A2_bc], out_final=True)
        c.dma('sp', out[256:384, 0:8], A1[:], r=[A1], out_final=True)
        c.dma('sp', out[256:384, 8:24], modT[:, :, :].rearrange("p a b -> p (a b)"), r=[modT], out_final=True)
        c.finish()
        return nc

    S1 = _Scope(c, "s1_")
    Win = S1.sb("Win", [128, 8, 2304], BF16)
    Wout = S1.sb("Wout", [128, 8, 1024], BF16)
    KAT = S1.sb("KAT", [128, 8192], BF16)
    VA = S1.sb("VA", [128, NBLK_ALL, 2, 65], BF16)
    xbuf = [S1.sb("x%d" % i, [128, 1024], F32) for i in range(2)]
    ropeb = [S1.sb("rope%d" % i, [128, 2, 64], F32) for i in range(2)]
    xn = S1.sb("xn", [128, 1024], BF16)
    hT = S1.sb("hT", [128, 8, 128], BF16)
    st1 = S1.sb("st1", [128, 16], F32)
    D = [S1.ps("D%d" % i, [128, 1024], F32) for i in range(4)]
    Dk = lambda i, h: "D%d%s" % (i, "ab"[h])

    w_in_v = w_in.rearrange("(j p) n -> p j n", p=128)
    for k in range(3):
        op('pool', lambda e: e.dma_start(out=Win[:, :, k * 768:(k + 1) * 768], in_=w_in_v[:, :, k * 768:(k + 1) * 768]),
           w=[Win], dma=True)
    op('pool', lambda e: e.dma_start(out=Wout[:], in_=w_out.rearrange("(j p) n -> p j n", p=128)), w=[Wout], dma=True)
    op('dve', lambda e: e.memset(VA[:, :, :, 64:65], 1.0), w=[VA])

    A1b = _bc(A1[:], 2, [128, 8, 128])
    B1b = _bc(modT[:, 0, :], 2, [128, 8, 128])
    pTv = D[0][:, 0:512].bitcast(BF16).rearrange("p (j n) -> p j n", j=8)

    def load_x(i):
        c.dma('sp', xbuf[i % 2][:], xs[i * 128:(i + 1) * 128, :], w=[xbuf[i % 2]])
        c.dma('sp', ropeb[i % 2][:], rope_d[i], w=[ropeb[i % 2]])

    def norm_hT_g(xt):
        op('act', lambda e: e.activation(xn[:], xt[:], AF.Square, accum_out=st1[:, 0:1]), r=[xt], w=[st1, xn]); yield
        op('dve', lambda e: e.tensor_scalar(st1[:, 1:2], st1[:, 0:1], 1.0 / D_MODEL, EPS, ALU.mult, ALU.add), r=[st1], w=[st1]); yield
        op('act', lambda e: e.activation(st1[:, 3:4], st1[:, 1:2], AF.Ln), r=[st1], w=[st1]); yield
        op('act', lambda e: e.activation(st1[:, 2:3], st1[:, 3:4], AF.Exp, scale=-0.5), r=[st1], w=[st1]); yield
        op('act', lambda e: e.activation(xn[:], xt[:], AF.Identity, scale=st1[:, 2:3]), r=[xt, st1], w=[xn]); yield
        for j in range(8):
            op('pe', lambda e: e.transpose(pTv[:, j, :], xn[:, j * 128:(j + 1) * 128], identb[:]), r=[xn, identb], w=["D0a"])
        yield
        op('dve', lambda e: e.tensor_tensor(hT[:], pTv, A1b, ALU.mult), r=["D0a", A1], w=[hT]); yield
        op('dve', lambda e: e.tensor_tensor(hT[:], hT[:], B1b, ALU.add), r=[hT, modT], w=[hT]); yield

    def norm_hT(xt):
        for _ in norm_hT_g(xt):
            pass

    def head_norm_rope_g(dst_bf, src_sb, H, gain, rp, scr):
        sq, ssq, tmp, t1, t2 = scr
        sv = src_sb[:, 0:H * 64].rearrange("p (h d) -> p h d", h=H)
        sqv = sq[:, 0:H * 64].rearrange("p (h d) -> p h d", h=H)
        op('dve', lambda e: e.tensor_tensor(sqv, sv, sv, ALU.mult), r=[src_sb], w=[sq])
        yield
        op('dve', lambda e: e.tensor_reduce(ssq[:, 0:H], sqv, AX.X, ALU.add), r=[sq], w=[ssq])
        yield
        op('dve', lambda e: e.tensor_scalar(ssq[:, 8:8 + H], ssq[:, 0:H], 1.0 / 64, EPS, ALU.mult, ALU.add), r=[ssq], w=[ssq])
        yield
        op('act', lambda e: e.activation(ssq[:, 0:H], ssq[:, 8:8 + H], AF.Ln), r=[ssq], w=[ssq])
        yield
        op('act', lambda e: e.activation(ssq[:, 16:16 + H], ssq[:, 0:H], AF.Exp, scale=-0.5), r=[ssq], w=[ssq])
        yield
        tv = tmp[:, 0:H * 64].rearrange("p (h d) -> p h d", h=H)
        op('dve', lambda e: e.tensor_tensor(tv, sv, _bc(ssq[:, 16:16 + H], 2, [128, H, 64]), ALU.mult), r=[src_sb, ssq], w=[tmp])
        yield
        op('dve', lambda e: e.tensor_tensor(tv, tv, _bc(gain[:], 1, [128, H, 64]), ALU.mult), r=[tmp, gain], w=[tmp])
        yield
        t1v = t1[:, 0:H * 64].rearrange("p (h d) -> p h d", h=H)
        op('dve', lambda e: e.tensor_tensor(t1v, tv, _bc(rp[:, 0, :], 1, [128, H, 64]), ALU.mult), r=[tmp, rp], w=[t1])
        yield
        x5 = tmp[:, 0:H * 64].rearrange("p (h a b d) -> p h a b d", h=H, a=2, b=2)
        o5 = t2[:, 0:H * 64].rearrange("p (h a b d) -> p h a b d", h=H, a=2, b=2)
        s5 = rp[:, 1, :].rearrange("p (a b d) -> p a b d", a=2, b=2)
        for b_ in range(2):
            op('dve', lambda e: e.tensor_tensor(o5[:, :, :, b_, :], x5[:, :, :, 1 - b_, :],
                                                _bc(s5[:, :, b_, :], 1, [128, H, 2, 16]), ALU.mult), r=[tmp, rp], w=[t2])
            yield
        op('dve', lambda e: e.tensor_tensor(dst_bf[:, 0:H * 64], t1[:, 0:H * 64], t2[:, 0:H * 64], ALU.add), r=[t1, t2], w=[dst_bf])
        yield


    def head_norm_rope(dst_bf, src_sb, H, gain, rp, scr):
        for _ in head_norm_rope_g(dst_bf, src_sb, H, gain, rp, scr):
            pass

    ka_sb = S1.sb("ka_sb", [128, 128], F32)
    scrA = (S1.sb("sqA", [128, 512], F32), S1.sb("ssqA", [128, 24], F32), S1.sb("tmpA", [128, 512], F32),
            S1.sb("t1A", [128, 512], F32), S1.sb("t2A", [128, 512], F32))
    kr = S1.sb("kr", [128, 128], BF16)
    kbs = [S1.sb("kbs%d" % i, [128, 4, 128], BF16) for i in range(2)]
    vbs = [S1.sb("vbs%d" % i, [128, 512], BF16) for i in range(2)]
    pA = D[0][:, 512:768]
    pKv = D[1][:, 0:64].bitcast(BF16)
    pKB = D[2][:, 0:512].rearrange("p (f n) -> p f n", f=4)
    pVB = D[3][:, 0:512]
    cvb = [S1.sb("cv%d" % i, [128, 2048], BF16) for i in range(2)]
    n_cv = 128
    per_blk = -(-n_cv // nkv)

    def convert(k):
        cv = cvb[k % 2]
        op('pool', lambda e: e.dma_start(out=cv[:], in_=peer_uv[k * 128:(k + 1) * 128, :]), w=[cv], dma=True)
        c.dma('sp', uvb[k * 128:(k + 1) * 128, :], cv[:], r=[cv])
    load_x(0)
    for i in range(nkv):
        if i + 1 < nkv:
            load_x(i + 1)
        for k in range(i * per_blk, min((i + 1) * per_blk, n_cv)):
            convert(k)
        xt = xbuf[i % 2]; rp = ropeb[i % 2]
        norm_hT(xt)
        for j in range(8):
            op('pe', lambda e: e.matmul(pA, hT[:, j, :], Win[:, j, 512:768], start=(j == 0), stop=(j == 7)), r=[hT, Win], w=["D0b"])
        op('act', lambda e: e.copy(ka_sb[:], pA[:, 0:128]), r=["D0b"], w=[ka_sb])
        op('act', lambda e: e.copy(VA[:, i, :, 0:64], pA[:, 128:256].rearrange("p (g d) -> p g d", g=2)), r=["D0b"], w=[VA])
        head_norm_rope(kr, ka_sb, 2, kg, rp, scrA)
        op('pe', lambda e: e.transpose(pKv, kr[:], identb[:]), r=[kr, identb], w=["D1a"])
        op('act', lambda e: e.copy(KAT[:, i * 128:(i + 1) * 128], pKv), r=["D1a"], w=[KAT])
        if i < NBLK_KVB:
            for f in range(4):
                for j in range(8):
                    op('pe', lambda e: e.matmul(pKB[:, f, :], Win[:, j, 1280 + f * 128:1280 + (f + 1) * 128], hT[:, j, :],
                                                start=(j == 0), stop=(j == 7)), r=[hT, Win], w=["D2a"])
            ks = kbs[i % 2]; vs = vbs[i % 2]
            op('act', lambda e: e.copy(ks[:], pKB), r=["D2a"], w=[ks])
            c.dma('sp', kbt_dram[i], ks[:], r=[ks])
            for j in range(8):
                op('pe', lambda e: e.matmul(pVB, hT[:, j, :], Win[:, j, 1792:2304], start=(j == 0), stop=(j == 7)), r=[hT, Win], w=["D3a"])
            op('dve', lambda e: e.tensor_copy(vs[:], pVB), r=["D3a"], w=[vs])
            c.dma('sp', vb_dram[i], vs[:], r=[vs])
    c.barrier()
    if stop_after == 'A':
        dbg = S1.sb("dbg", [128, 1024], F32)
        c.op('dve', lambda e: e.tensor_copy(dbg[:, 0:256], KAT[:, 0:256]), r=[KAT], w=[dbg])
        c.op('dve', lambda e: e.tensor_copy(dbg[:, 256:516], VA[:, 0:2, :, :].rearrange("p a g d -> p (a g d)")), r=[VA], w=[dbg])
        c.dma('sp', out[0:128, :], dbg[:], r=[dbg], out_final=True)
        dbg2 = S1.sb("dbg2", [128, 1024], BF16)
        c.dma('sp', dbg2[:, 0:512], kbt_dram[1].rearrange("p f t -> p (f t)"), w=[dbg2])
        c.dma('sp', dbg2[:, 512:1024], vb_dram[1], w=[dbg2])
        dbg3 = S1.sb("dbg3", [128, 1024], F32)
        c.op('dve', lambda e: e.tensor_copy(dbg3[:], dbg2[:]), r=[dbg2], w=[dbg3])
        c.dma('sp', out[128:256, :], dbg3[:], r=[dbg3], out_final=True)
        c.finish()
        return nc

    kwin = [S1.sb("kwin%d" % i, [128, 5, 4, 128], BF16) for i in range(1)]
    vwin = [S1.sb("vwin%d" % i, [128, 5, 512], BF16) for i in range(1)]
    nbt = S1.sb("nbt", [128, 8, 640], F32)
    qa_sb = S1.sb("qa_sb", [128, 512], F32)
    qr = S1.sb("qr", [128, 512], BF16)
    qb = S1.sb("qb", [128, 512], BF16)
    qAT = [S1.sb("qAT%d" % i, [128, 4, 128], BF16) for i in range(2)]
    qBTs = [S1.sb("qBT%d" % i, [128, 4, 128], BF16) for i in range(2)]
    PT2 = [S1.sb("PT2_%d" % i, [128, 1024], BF16) for i in range(3)]
    oT = S1.sb("oT", [65, 2, 512], F32)
    st2 = S1.sb("st2", [128, 48], F32)
    mixf = S1.sb("mixf", [128, 1024], F32)
    mixn = S1.sb("mixn", [128, 1024], BF16)
    mixT = S1.sb("mixT", [128, 8, 128], BF16)
    s_sb = S1.sb("s_sb", [128, 640], F32)
    p_sb = S1.sb("p_sb", [128, 640], BF16)
    PTn = S1.sb("PTn", [128, 5, 128], BF16)
    x1t = [S1.sb("x1t%d" % i, [128, 1024], F32) for i in range(1)]

    def win_start(j):
        return min(max(2 * j - 4, 0), 58) // 2

    def load_win(j):
        cb = win_start(j)
        c.dma('sp', kwin[0][:], kbt_dram[cb:cb + 5].rearrange("b p f t -> p b f t"), w=[kwin[0]])
        c.dma('sp', vwin[0][:], vb_dram[cb:cb + 5].rearrange("b p n -> p b n"), w=[vwin[0]])

    Dp = [D[1], D[2], D[3]]
    Dpk = [("D1a", "D1b"), ("D2a", "D2b"), ("D3a", "D3b")]
    pO = [D[0][0:65, 0:512], D[0][0:65, 512:1024]]
    pOk = ["D0a", "D0b"]
    pT2 = D[0][:, 512:1024].bitcast(BF16).rearrange("p (j n) -> p j n", j=8)
    pTa = D[1][:, :].rearrange("p (s n) -> p s n", s=8)
    import os
    print("B1 start n_ins", c.n_ins)
    if os.environ.get("CUT"):
        c.cut = c.n_ins + int(os.environ["CUT"])
    for g_ in range(2):
        op('dve', lambda e: e.memset(qAT[g_][:], 0.0), w=[qAT[g_]])
    def pre_gen(jb):
        xt_ = xbuf[jb % 2]; rp_ = ropeb[jb % 2]; qBT_ = qBTs[jb % 2]
        yield from norm_hT_g(xt_)
        for hh, c0 in ((0, 0), (1, 768)):
            for jj in range(8):
                op('pe', lambda e: e.matmul(D[3][:, hh * 512:(hh + 1) * 512], hT[:, jj, :], Win[:, jj, c0:c0 + 512],
                                            start=(jj == 0), stop=(jj == 7)), r=[hT, Win], w=[Dk(3, hh)])
            yield
        op('act', lambda e: e.copy(qa_sb[:], D[3][:, 0:512]), r=["D3a"], w=[qa_sb]); yield
        op('act', lambda e: e.copy(qb[:], D[3][:, 512:1024]), r=["D3b"], w=[qb]); yield
        yield from head_norm_rope_g(qr, qa_sb, 8, qg, rp_, scrA)
        for k in range(4):
            op('pe', lambda e: e.transpose(pT2[:, k, :], qr[:, k * 128:(k + 1) * 128], identb[:]), r=[qr, identb], w=["D0b"])
        for k in range(4):
            op('pe', lambda e: e.transpose(pT2[:, 4 + k, :], qb[:, k * 128:(k + 1) * 128], identb[:]), r=[qb, identb], w=["D0b"])
        yield
        for g_ in range(2):
            op('dve', lambda e: e.tensor_copy(qAT[g_][64 * g_:64 * g_ + 64, :, :], pT2[64 * g_:64 * g_ + 64, 0:4, :]), r=["D0b"], w=[qAT[g_]])
            yield
        op('dve', lambda e: e.tensor_copy(qBT_[:], pT2[:, 4:8, :]), r=["D0b"], w=[qBT_]); yield

    def _drain(g):
        if g is not None:
            for _ in g:
                pass

    def _step(g, n):
        if g is not None:
            for _ in range(n):
                try:
                    next(g)
                except StopIteration:
                    return

    if nown > 0:
        load_x(0)
        if not os.environ.get("NO_WIN"):
            load_win(0)
        _drain(pre_gen(0))
    for j in range(nown):
        xt = xbuf[j % 2]; rp = ropeb[j % 2]; qBT = qBTs[j % 2]
        if j + 1 < nown:
            load_x(j + 1)
        if j <= 2 and not os.environ.get("NO_NBT"):
            c.dma('sp', nbt[:], nbias_d[j], w=[nbt])
        for g in range(2):
            pb = 64 * g
            npair = nkv // 2

            def S2(pi):
                for u in range(2):
                    kc = 2 * pi + u
                    op('pe', lambda e: e.matmul(Dp[pi % 3][:, u * 512:(u + 1) * 512], KAT[:, kc * 128:(kc + 1) * 128],
                                                qAT[g][:, :, :], start=True, stop=True), r=[KAT, qAT[g]], w=[Dpk[pi % 3][u]])

            def EXP2(pi):
                op('act', lambda e: e.activation(PT2[pi % 3][:], Dp[pi % 3][:, :], AF.Exp, scale=0.125, bias=negC[:, 0:1]),
                   r=[Dpk[pi % 3][0], Dpk[pi % 3][1], negC], w=[PT2[pi % 3]])

            def PV2(pi):
                for u in range(2):
                    kc = 2 * pi + u
                    op('pe', lambda e: e.matmul(pO[g], VA[:, kc, g, :], PT2[pi % 3][:, u * 512:(u + 1) * 512],
                                                start=(kc == 0), stop=(kc == nkv - 1)), r=[VA, PT2[pi % 3]], w=[pOk[g]])
            S2(0)
            if npair > 1:
                S2(1)
            for pi in range(npair):
                EXP2(pi)
                if pi + 2 < npair:
                    S2(pi + 2)
                PV2(pi)
            op('dve', lambda e: e.tensor_copy(oT[:, g, :], pO[g]), r=[pOk[g]], w=[oT])
        for g in range(2):
            for i in range(4):
                op('pe', lambda e: e.transpose(pTa[:, g * 4 + i, 0:65], oT[0:65, g, i * 128:(i + 1) * 128], identf[0:65, 0:65]),
                   r=[oT, identf], w=["D1a", "D1b"])
        op('dve', lambda e: e.reciprocal(st2[:, 0:8], pTa[:, :, 64]), r=["D1a", "D1b"], w=[st2])
        op('dve', lambda e: e.tensor_tensor(mixf[:, 0:512].rearrange("p (s d) -> p s d", s=8), pTa[:, :, 0:64],
                                            _bc(st2[:, 0:8], 2, [128, 8, 64]), ALU.mult), r=["D1a", "D1b", st2], w=[mixf])
        if dbg == 2:
            c.dma('sp', out[0:128, :], mixf[:], r=[mixf], out_final=True)
            c.finish()
            return nc
        kw = kwin[0]; vw = vwin[0]
        pNs = [D[2], D[2]]
        pNk = [("D2a", "D2b"), ("D2a", "D2b")]
        nxt_pre = pre_gen(j + 1) if j + 1 < nown else None
        pPT = D[1][:, 0:320].bitcast(BF16).rearrange("p (c n) -> p c n", c=5)
        pOB = D[1][:, 512:1024]
        s_sbs = [s_sb[:], cvb[0][:, :].bitcast(F32)[:, 0:640]]
        s_sbk = [s_sb, cvb[0]]
        p_sbs = [p_sb[:], cvb[1][:, 0:640]]
        p_sbk = [p_sb, cvb[1]]

        def na_front(h):
            pr, hb = h // 2, 64 * (h % 2)
            pN = pNs[h % 2]; ss_ = s_sbs[h % 2]; ps_ = p_sbs[h % 2]
            op('pe', lambda e: e.matmul(pN[:, 0:512], qBT[hb:hb + 64, pr, :], kw[hb:hb + 64, 0:4, pr, :], start=True, stop=True),
               r=[qBT, kw], w=[pNk[h % 2][0]])
            op('pe', lambda e: e.matmul(pN[:, 512:640], qBT[hb:hb + 64, pr, :], kw[hb:hb + 64, 4, pr, :], start=True, stop=True),
               r=[qBT, kw], w=[pNk[h % 2][1]])
            op('dve', lambda e: e.scalar_tensor_tensor(ss_, pN[:, 0:640], 0.125, nbt[:, h, :], ALU.mult, ALU.add),
               r=[pNk[h % 2][0], pNk[h % 2][1], nbt], w=[s_sbk[h % 2]])
            op('dve', lambda e: e.tensor_reduce(st2[:, 8 + h % 2:9 + h % 2], ss_, AX.X, ALU.max, negate=True), r=[s_sbk[h % 2]], w=["st2_nm%d" % (h % 2)])
            op('act', lambda e: e.activation(ps_, ss_, AF.Exp, bias=st2[:, 8 + h % 2:9 + h % 2], accum_out=st2[:, 16 + h:17 + h]),
               r=[s_sbk[h % 2], "st2_nm%d" % (h % 2)], w=[p_sbk[h % 2], "st2_rs%d" % h])
        na_front(0)
        for h in range(8):
            if h + 1 < 8:
                na_front(h + 1)
            ps_ = p_sbs[h % 2]
            for cc in range(5):
                op('pe', lambda e: e.transpose(pPT[:, cc, :], ps_[:, cc * 128:(cc + 1) * 128], identb[:]), r=[p_sbk[h % 2], identb], w=["D1a"])
            op('dve', lambda e: e.tensor_copy(PTn[:], pPT), r=["D1a"], w=[PTn])
            for cc in range(5):
                op('pe', lambda e: e.matmul(pOB[:, h * 64:(h + 1) * 64], PTn[:, cc, :], vw[:, cc, h * 64:(h + 1) * 64],
                                            start=(cc == 0), stop=(cc == 4)), r=[PTn, vw], w=["D1b"])
            _step(nxt_pre, 4)
        _drain(nxt_pre)
        if j + 1 < nown:
            load_win(j + 1)
        op('dve', lambda e: e.reciprocal(st2[:, 24:32], st2[:, 16:24]), r=["st2_rs%d" % h_ for h_ in range(8)], w=["st2_ri"])
        op('dve', lambda e: e.tensor_tensor(mixf[:, 512:1024].rearrange("p (s d) -> p s d", s=8),
                                            pOB.rearrange("p (s d) -> p s d", s=8),
                                            _bc(st2[:, 24:32], 2, [128, 8, 64]), ALU.mult), r=["D1b", "st2_ri"], w=[mixf])
        if dbg == 3:
            c.dma('sp', out[0:128, :], mixf[:], r=[mixf], out_final=True)
            c.finish()
            return nc
        for gi in range(2):
            op('dve', lambda e: e.scalar_tensor_tensor(s_sb[:, 0:512], mixf[:, gi * 512:(gi + 1) * 512], 1.0, mixf[:, gi * 512:(gi + 1) * 512],
                                                       ALU.mult, ALU.mult, accum_out=st2[:, 32 + gi:33 + gi]), r=[mixf], w=[st2, s_sb])
        op('dve', lambda e: e.tensor_scalar(st2[:, 34:36], st2[:, 32:34], 1.0 / 512, EPS, ALU.mult, ALU.add), r=[st2], w=[st2])
        op('act', lambda e: e.activation(st2[:, 38:40], st2[:, 34:36], AF.Ln), r=[st2], w=[st2])
        op('act', lambda e: e.activation(st2[:, 36:38], st2[:, 38:40], AF.Exp, scale=-0.5), r=[st2], w=[st2])
        for gi in range(2):
            op('act', lambda e: e.activation(mixn[:, gi * 512:(gi + 1) * 512], mixf[:, gi * 512:(gi + 1) * 512], AF.Identity,
                                             scale=st2[:, 36 + gi:37 + gi]), r=[mixf, st2], w=[mixn])
        for k in range(8):
            op('pe', lambda e: e.transpose(pTv[:, k, :], mixn[:, k * 128:(k + 1) * 128], identb[:]), r=[mixn, identb], w=["D0a"])
        op('dve', lambda e: e.tensor_tensor(mixT[:], pTv, _bc(gnT[:], 2, [128, 8, 128]), ALU.mult), r=["D0a", gnT], w=[mixT])
        for hh in range(2):
            for jj in range(8):
                op('pe', lambda e: e.matmul(D[1][:, hh * 512:(hh + 1) * 512], mixT[:, jj, :], Wout[:, jj, hh * 512:(hh + 1) * 512],
                                            start=(jj == 0), stop=(jj == 7)), r=[mixT, Wout], w=[Dk(1, hh)])
        xo = x1t[0]
        op('dve', lambda e: e.tensor_tensor(mixf[:], D[1][:, :], gate1_bc[:], ALU.mult), r=["D1a", "D1b", gate1_bc], w=[mixf])
        op('dve', lambda e: e.tensor_tensor(xo[:], mixf[:], xt[:], ALU.add), r=[mixf, xt], w=[xo])
        c.dma('sp', out[j * 128:(j + 1) * 128, :], xo[:], r=[xo], out_final=True)
    c.barrier()
    S1.close()
    if stop_after == 'B1':
        c.finish()
        return nc
    _phase_c(c, nc, nown, out, w_q, uvb, k1T, k2T, identb, neghalf, iota16, gate2_bc, A2_bc, B2_bc, gf_bc)
    c.finish()
    return nc


NB = 22
GS = 4
LEAD = 14


def _phase_c(c, nc, nown, out, w_q, uvb, k1T, k2T, identb, neghalf, iota16, gate2_bc, A2_bc, B2_bc, gf_bc, pipeline=True):
    op = c.op
    S2 = _Scope(c, "s2_")
    Wq = S2.sb("Wq", [128, 8, 2048], BF16)
    w_q_v = w_q.rearrange("(j p) n -> p j n", p=128)
    for k in range(2):
        op('pool', lambda e: e.dma_start(out=Wq[:, :, k * 1024:(k + 1) * 1024], in_=w_q_v[:, :, k * 1024:(k + 1) * 1024]),
           w=[Wq], dma=True)
    x1b = [S2.sb("x1b%d" % i, [128, 1024], F32) for i in range(2)]
    h2s = [S2.sb("h2_%d" % i, [128, 1024], F32) for i in range(1)]
    h2bs = [S2.sb("h2b%d" % i, [128, 1024], BF16) for i in range(2)]
    prod = [S2.sb("prod%d" % i, [128, 1024], BF16) for i in range(4)]
    eidxs = [S2.sb("eidx%d" % i, [128, 128], I32) for i in range(2)]
    gws = [S2.sb("gw%d" % i, [128, 8, 16], F32) for i in range(2)]
    st = S2.sb("st", [128, 16], F32)
    stt = S2.sb("stt", [128, 16], F32)
    xn2 = S2.sb("xn2", [128, 1024], F32)
    h2T = S2.sb("h2T", [128, 8, 128], BF16)
    qT = S2.sb("qT", [128, 16, 128], BF16)
    s_sb = S2.sb("s_sb", [128, 16, 128], F32)
    wk = S2.sb("wk", [128, 256], F32)
    vals = S2.sb("vals", [128, 16, 16], F32)
    idxs = S2.sb("idxs", [128, 16, 16], U32)
    idxf = S2.sb("idxf", [128, 16, 16], F32)
    cand = S2.sb("cand", [128, 8, 256], F32)
    eq = cand
    eqv = cand[:, :, :].rearrange("p h (k m) -> p h k m", k=16)
    tv = S2.sb("tv", [128, 8, 16], F32)
    tp = S2.sb("tp", [128, 8, 16], U32)
    ij = S2.sb("ij", [128, 2, 128], U32)
    ijf = S2.sb("ijf", [128, 2, 128], F32)
    I12 = S2.sb("I12", [128, 2, 128], F32)
    eidf = S2.sb("eidf", [128, 128], F32)
    sm = S2.sb("sm", [128, 16], F32)
    a_t = S2.sb("a_t", [128, 128], F32)
    ga = S2.sb("ga", [128, 128], F32)
    wgt = S2.sb("wgt", [128, 128], F32)
    gb = [S2.sb("gb%d" % i, [128, 2048], BF16) for i in range(NB)]
    dg = [S2.sb("dg%d" % i, [128, 128], BF16) for i in range(4)]
    yt = h2s[0]
    xo = xn2
    D = [S2.ps("D%d" % i, [128, 1024], F32) for i in range(4)]
    pTv = D[0][:, 0:512].bitcast(BF16).rearrange("p (j n) -> p j n", j=8)
    pacc = D[3]

    def load(j):
        c.dma('sp', x1b[j % 2][:], out[j * 128:(j + 1) * 128, :], w=[x1b[j % 2]])

    def front(j):
        x1 = x1b[j % 2]; h2 = h2s[0]; h2b = h2bs[j % 2]; eidx = eidxs[j % 2]; gw = gws[j % 2]
        op('act', lambda e: e.activation(xn2[:], x1[:], AF.Square, accum_out=st[:, 0:1]), r=[x1], w=[st, xn2]); yield
        op('dve', lambda e: e.tensor_scalar(st[:, 1:2], st[:, 0:1], 1.0 / D_MODEL, EPS, ALU.mult, ALU.add), r=[st], w=[st]); yield
        op('act', lambda e: e.activation(st[:, 3:4], st[:, 1:2], AF.Ln), r=[st], w=[st])
        op('act', lambda e: e.activation(st[:, 2:3], st[:, 3:4], AF.Exp, scale=-0.5), r=[st], w=[st])
        op('act', lambda e: e.activation(xn2[:], x1[:], AF.Identity, scale=st[:, 2:3]), r=[x1, st], w=[xn2]); yield
        op('dve', lambda e: e.tensor_tensor(h2[:], xn2[:], A2_bc[:], ALU.mult), r=[xn2, A2_bc], w=[h2]); yield
        op('dve', lambda e: e.tensor_tensor(h2[:], h2[:], B2_bc[:], ALU.add), r=[h2, B2_bc], w=[h2]); yield
        op('act', lambda e: e.copy(h2b[:], h2[:]), r=[h2], w=[h2b])
        for k in range(8):
            op('pe', lambda e: e.transpose(pTv[:, k, :], h2b[:, k * 128:(k + 1) * 128], identb[:]), r=[h2b, identb], w=["E0a"])
        op('dve', lambda e: e.tensor_copy(h2T[:], pTv), r=["E0a"], w=[h2T]); yield
        for hf in range(2):
            dq = D[2][:, :].rearrange("p (s n) -> p s n", s=8)
            for f8 in range(8):
                f = hf * 8 + f8
                for jj in range(8):
                    op('pe', lambda e: e.matmul(dq[:, f8, :], Wq[:, jj, f * 128:(f + 1) * 128], h2T[:, jj, :],
                                                start=(jj == 0), stop=(jj == 7)), r=[Wq, h2T], w=["E2"])
            op('act', lambda e: e.copy(qT[:, hf * 8:(hf + 1) * 8, :], dq), r=["E2"], w=[qT]); yield
        for hf in range(2):
            ds = D[1][:, :].rearrange("p (s n) -> p s n", s=8)
            for f8 in range(8):
                f = hf * 8 + f8
                kk = k1T if f % 2 == 0 else k2T
                op('pe', lambda e: e.matmul(ds[:, f8, :], qT[:, f, :], kk[:], start=True, stop=True), r=[qT, kk], w=["E1"])
            op('act', lambda e: e.copy(s_sb[:, hf * 8:(hf + 1) * 8, :], ds), r=["E1"], w=[s_sb]); yield
        for f in range(16):
            op('dve', lambda e: e.max(vals[:, f, 0:8], s_sb[:, f, :]), r=[s_sb], w=[vals]); yield
            op('dve', lambda e: e.max_index(idxs[:, f, 0:8], vals[:, f, 0:8], s_sb[:, f, :]), r=[s_sb, vals], w=[idxs]); yield
            op('dve', lambda e: e.match_replace(wk[:, 0:128], vals[:, f, 0:8], s_sb[:, f, :], -1e30), r=[s_sb, vals], w=[wk]); yield
            op('dve', lambda e: e.max(vals[:, f, 8:16], wk[:, 0:128]), r=[wk], w=[vals]); yield
            op('dve', lambda e: e.max_index(idxs[:, f, 8:16], vals[:, f, 8:16], wk[:, 0:128]), r=[wk, vals], w=[idxs]); yield
        v4 = vals[:, :, :].rearrange("p (h a) k -> p h a k", a=2)
        op('dve', lambda e: e.tensor_tensor(cand[:, :, :].rearrange("p h (i k) -> p h i k", i=16),
                                            _bc(v4[:, :, 0, :], 3, [128, 8, 16, 16]), _bc(v4[:, :, 1, :], 2, [128, 8, 16, 16]), ALU.add),
           r=[vals], w=[cand]); yield
        for h in range(8):
            op('dve', lambda e: e.max(tv[:, h, 0:8], cand[:, h, :]), r=[cand], w=[tv]); yield
            op('dve', lambda e: e.max_index(tp[:, h, 0:8], tv[:, h, 0:8], cand[:, h, :]), r=[cand, tv], w=[tp]); yield
            op('dve', lambda e: e.match_replace(wk[:], tv[:, h, 0:8], cand[:, h, :], -1e30), r=[cand, tv], w=[wk]); yield
            op('dve', lambda e: e.max(tv[:, h, 8:16], wk[:]), r=[wk], w=[tv]); yield
            op('dve', lambda e: e.max_index(tp[:, h, 8:16], tv[:, h, 8:16], wk[:]), r=[wk, tv], w=[tp]); yield
        tpf = tp[:, :, :].rearrange("p h k -> p (h k)")
        op('dve', lambda e: e.tensor_single_scalar(ij[:, 0, :], tpf, 4, ALU.logical_shift_right), r=[tp], w=[ij]); yield
        op('dve', lambda e: e.tensor_single_scalar(ij[:, 1, :], tpf, 15, ALU.bitwise_and), r=[tp, ij], w=[ij]); yield
        op('dve', lambda e: e.tensor_copy(ijf[:], ij[:]), r=[ij], w=[ijf]); yield
        op('dve', lambda e: e.tensor_copy(idxf[:], idxs[:]), r=[idxs], w=[idxf]); yield
        i4 = idxf[:, :, :].rearrange("p (h a) k -> p h a k", a=2)
        iot = iota16[:, :].unsqueeze(1).unsqueeze(1).to_broadcast([128, 8, 16, 16])
        for a in range(2):
            sel = ijf[:, a, :].rearrange("p (h k) -> p h k", h=8)
            op('dve', lambda e: e.tensor_tensor(eqv, _bc(sel, 3, [128, 8, 16, 16]), iot, ALU.is_equal), r=[ijf, iota16], w=[eq]); yield
            op('dve', lambda e: e.tensor_tensor(eqv, eqv, _bc(i4[:, :, a, :], 2, [128, 8, 16, 16]), ALU.mult), r=[eq, idxf], w=[eq]); yield
            op('dve', lambda e: e.tensor_reduce(I12[:, a, :].rearrange("p (h k) -> p h k", h=8), eqv, AX.X, ALU.add), r=[eq], w=[I12]); yield
        op('dve', lambda e: e.scalar_tensor_tensor(eidf[:], I12[:, 0, :], 128.0, I12[:, 1, :], ALU.mult, ALU.add), r=[I12], w=[eidf]); yield
        op('dve', lambda e: e.tensor_copy(eidx[:], eidf[:]), r=[eidf], w=[eidx]); yield
        op('dve', lambda e: e.tensor_tensor(gw[:], tv[:], _bc(tv[:, :, 0], 2, [128, 8, 16]), ALU.subtract), r=[tv], w=[gw]); yield
        op('act', lambda e: e.activation(gw[:], gw[:], AF.Exp), r=[gw], w=[gw])
        op('dve', lambda e: e.tensor_reduce(sm[:, 0:8], gw[:], AX.X, ALU.add), r=[gw], w=[sm]); yield
        op('dve', lambda e: e.reciprocal(sm[:, 8:16], sm[:, 0:8]), r=[sm], w=[sm]); yield
        op('dve', lambda e: e.tensor_tensor(gw[:], gw[:], _bc(sm[:, 8:16], 2, [128, 8, 16]), ALU.mult), r=[gw, sm], w=[gw]); yield

    def drain(g):
        if g is not None:
            for _ in g:
                pass

    def step(g, n=1):
        if g is not None:
            for _ in range(n):
                try:
                    next(g)
                except StopIteration:
                    return

    if nown > 0:
        load(0)
        drain(front(0))
    for j in range(nown):
        nxt = None
        if j + 1 < nown:
            load(j + 1)
            nxt = front(j + 1)
            if not pipeline:
                drain(nxt); nxt = None
        x1 = x1b[j % 2]; h2b = h2bs[j % 2]; eidx = eidxs[j % 2]; gw = gws[j % 2]
        gwf = gw[:, :, :].rearrange("p h k -> p (h k)")

        def gather(s, jb=j):
            b = gb[s % NB]
            ei = eidxs[jb % 2]
            op('pool', lambda e: e.indirect_dma_start(out=b[:], out_offset=None, in_=uvb,
                                                      in_offset=bass.IndirectOffsetOnAxis(ap=ei[:, s:s + 1], axis=0)),
               r=[ei], w=[b], dma=True)
        def finish(g0):
            gs_ = slice(g0, g0 + GS)
            op('dve', lambda e: e.tensor_tensor(wgt[:, gs_], ga[:, gs_], gwf[:, gs_], ALU.mult), r=[ga, gw], w=[wgt])
            for s in range(g0, g0 + GS):
                b = gb[s % NB]; d = dg[s % 4]
                if True:
                    op('act', lambda e: e.activation(d[:], identb[:], AF.Identity, scale=wgt[:, s:s + 1]), r=[identb, wgt], w=[d])
                else:
                    op('dve', lambda e: e.tensor_scalar(d[:], identb[:], wgt[:, s:s + 1], None, ALU.mult), r=[identb, wgt], w=[d])
                for hf in range(2):
                    op('pe', lambda e: e.matmul(pacc[:, hf * 512:(hf + 1) * 512], d[:], b[:, 1024 + hf * 512:1024 + (hf + 1) * 512],
                                                start=(s == 0), stop=(s == 127)), r=[d, b], w=["E3"])
        if j == 0:
            for s in range(LEAD):
                gather(s)
        prev = None
        for g0 in range(0, 128, GS):
            for s in range(g0, g0 + GS):
                if s + LEAD < 128:
                    gather(s + LEAD)
                b = gb[s % NB]
                pr_ = prod[s % 4]
                if s % GS == GS - 1:
                    op('dve', lambda e: e.scalar_tensor_tensor(pr_[:], b[:, 0:1024], 1.0, h2b[:], ALU.mult, ALU.mult,
                                                               accum_out=a_t[:, s:s + 1]), r=[b, h2b], w=[pr_, "a_t_dve"])
                else:
                    op('dve', lambda e: e.tensor_tensor(pr_[:], b[:, 0:1024], h2b[:], ALU.mult), r=[b, h2b], w=[pr_])
                    op('act', lambda e: e.activation(pr_[:], pr_[:], AF.Identity, accum_out=a_t[:, s:s + 1]),
                       r=[pr_], w=([pr_, "a_t_act"] if s % GS == GS - 2 else [pr_]))
                step(nxt, 1 + (s % 2))
            gs_ = slice(g0, g0 + GS)
            op('act', lambda e: e.activation(ga[:, gs_], a_t[:, gs_], AF.Gelu), r=["a_t_act", "a_t_dve"], w=[ga])
            if prev is not None:
                finish(prev)
            prev = g0
        finish(prev)
        drain(nxt)
        if j + 1 < nown:
            for s in range(LEAD):
                gather(s, j + 1)
        op('dve', lambda e: e.tensor_tensor(yt[:], pacc[:, :], gate2_bc[:], ALU.mult), r=["E3", gate2_bc], w=[yt])
        op('dve', lambda e: e.tensor_tensor(yt[:], yt[:], x1[:], ALU.add), r=[yt, x1], w=[yt])
        op('act', lambda e: e.activation(xo[:], yt[:], AF.Square, accum_out=stt[:, 4:5]), r=[yt], w=[stt, xo])
        op('dve', lambda e: e.tensor_scalar(stt[:, 5:6], stt[:, 4:5], 1.0 / D_MODEL, EPS, ALU.mult, ALU.add), r=[stt], w=[stt])
        op('act', lambda e: e.activation(stt[:, 7:8], stt[:, 5:6], AF.Ln), r=[stt], w=[stt])
        op('act', lambda e: e.activation(stt[:, 6:7], stt[:, 7:8], AF.Exp, scale=-0.5), r=[stt], w=[stt])
        op('act', lambda e: e.activation(xo[:], yt[:], AF.Identity, scale=stt[:, 6:7]), r=[yt, stt], w=[xo])
        op('dve', lambda e: e.tensor_tensor(yt[:], xo[:], gf_bc[:], ALU.mult), r=[xo, gf_bc], w=[yt])
        c.dma('sp', out[j * 128:(j + 1) * 128, :], yt[:], r=[yt], out_final=True)
    c.barrier()
    S2.close()


_ROPE_CACHE = {}


def _rope_tables(half):
    if half in _ROPE_CACHE:
        return _ROPE_CACHE[half]
    t = np.arange(8192)
    rho = t // 64
    col = t % 64
    row = rho if half == 0 else 127 - rho
    freqs = (np.float32(10000.0) ** (-np.arange(16, dtype=np.float32) / np.float32(16))).astype(np.float32)
    ar = row.astype(np.float32)[:, None] * freqs[None, :]
    ac = col.astype(np.float32)[:, None] * freqs[None, :]
    cr, sr, cc, sc = np.cos(ar), np.sin(ar), np.cos(ac), np.sin(ac)
    cos64 = np.concatenate([cr, cr, cc, cc], axis=1)
    sin64 = np.concatenate([-sr, sr, -sc, sc], axis=1)
    tab = np.stack([cos64, sin64], axis=1).astype(np.float32).reshape(64, 128, 2, 64)
    _ROPE_CACHE[half] = np.ascontiguousarray(tab)
    return _ROPE_CACHE[half]


def _nbias_tables(rpb, half):
    out = np.full((3, 128, 8, 640), NEG, dtype=np.float32)
    p = np.arange(128)
    a, cq = p // 64, p % 64
    kk = np.arange(640)
    kro, ck = kk // 64, kk % 64
    for j in range(3):
        w0 = min(max(2 * j - 4, 0), 58)
        rho = 2 * j + a
        kap = w0 + kro
        if half == 0:
            r, kr = rho, kap
        else:
            r, kr = 127 - rho, 127 - kap
        rs = np.clip(r - 4, 0, 120)
        cs = np.clip(cq - 8, 0, 48)
        vr = (kr[None, :] >= rs[:, None]) & (kr[None, :] <= rs[:, None] + 7)
        vc = (ck[None, :] >= cs[:, None]) & (ck[None, :] <= cs[:, None] + 15)
        valid = vr & vc
        dri = np.clip(kr[None, :] - r[:, None] + 7, 0, 14)
        dci = np.clip(ck[None, :] - cq[:, None] + 15, 0, 30)
        g = rpb[:, dri, dci]
        g = np.transpose(g, (1, 0, 2))
        out[j] = np.where(valid[:, None, :], g, np.float32(NEG))
    return out


_QPERM = np.array([(4 * g + i) * 64 + d for i in range(4) for g in range(2) for d in range(64)])


def _fm(v):
    return np.ascontiguousarray(np.asarray(v, dtype=np.float32).reshape(8, 128).T)


def make_in_maps(inputs):
    f = lambda k: np.asarray(inputs[k], dtype=np.float32)
    x = f("x"); cc = f("c")
    w_in = f("w_in")[0].copy()
    w_in[:, :512] = w_in[:, _QPERM]
    gn = np.concatenate([f("group_norm_a_g")[0], f("group_norm_b_g")[0]])
    shared = {
        "w_ada": np.ascontiguousarray(f("w_ada")[0]), "b_ada": np.ascontiguousarray(f("b_ada")[0][None, :]),
        "g1T": _fm(f("norm1_g")[0]), "g2row": np.ascontiguousarray(f("norm2_g")[0][None, :]),
        "gfrow": np.ascontiguousarray(f("final_norm_g")[None, :]), "w_in": np.ascontiguousarray(w_in),
        "qg": np.ascontiguousarray(f("q_norm_g")[0][None, :]), "kg": np.ascontiguousarray(f("k_norm_g")[0][None, :]),
        "gnT": _fm(gn), "w_out": np.ascontiguousarray(f("w_out")[0]), "w_q": np.ascontiguousarray(f("peer_w_query")[0]),
        "k1T": np.ascontiguousarray(f("peer_sub_keys_1")[0].T), "k2T": np.ascontiguousarray(f("peer_sub_keys_2")[0].T),
        "peer_uv": np.ascontiguousarray(np.stack([f("peer_u")[0], f("peer_v")[0]], axis=1).reshape(16384, 2048)),
    }
    rpb = f("natten_rpb")[0]
    nb = [_nbias_tables(rpb, h) for h in range(2)]
    maps = []
    for core in range(8):
        b, half = core // 2, core % 2
        xb = x[b]
        if half == 1:
            xb = xb.reshape(128, 64, 1024)[::-1].reshape(8192, 1024)
        m = dict(shared)
        m["xs"] = np.ascontiguousarray(xb)
        m["cT"] = _fm(cc[b])
        m["rope"] = _rope_tables(half)
        m["nbias"] = nb[half]
        maps.append(m)
    return maps


def assemble(results):
    out = np.empty((4, 8192, 1024), dtype=np.float32)
    for core in range(8):
        b, half = core // 2, core % 2
        o = np.asarray(results[core]["out"], dtype=np.float32).reshape(64, 64, 1024)
        if half == 0:
            out[b, :4096] = o.reshape(4096, 1024)
        else:
            out[b].reshape(128, 64, 1024)[64:] = o[::-1]
    return out


_NC_CACHE = {}


def kernel(**inputs):
    if "nc" not in _NC_CACHE:
        _NC_CACHE["nc"] = build_nc()
    nc = _NC_CACHE["nc"]
    maps = make_in_maps(inputs)
    res = run_bass_kernel_spmd(nc, maps, core_ids=list(range(8)))
    return assemble(res.results)
```

```python
import numpy as np
import ml_dtypes
import concourse.bass as bass
import concourse.mybir as mybir
from concourse.bass_utils import run_bass_kernel_spmd

F32 = mybir.dt.float32
BF16 = mybir.dt.bfloat16
I32 = mybir.dt.int32
U32 = mybir.dt.uint32
AF = mybir.ActivationFunctionType
ALU = mybir.AluOpType
AX = mybir.AxisListType


class Ctx:
    def __init__(self, nc):
        self.nc = nc
        self.eng = {'pe': nc.tensor, 'dve': nc.vector, 'act': nc.scalar, 'pool': nc.gpsimd, 'sp': nc.sync}
        self.sem = {k: nc.alloc_semaphore("s_" + k) for k in self.eng}
        self.cnt = {k: 0 for k in self.eng}
        self.know = {k: {} for k in self.eng}
        self.clock = {}
        self.last_w = {}
        self.readers = {}
        self.chan = {}
        self.names = {}
        self.n_wait = 0
        self.n_ins = 0
        self.out_events = []

    def sb(self, name, shape, dt):
        t = self.nc.alloc_sbuf_tensor(name, list(shape), dt)
        self.names[id(t)] = name
        return t

    def ps(self, name, shape, dt=F32):
        t = self.nc.alloc_psum_tensor(name, list(shape), dt)
        self.names[id(t)] = name
        return t

    def key(self, k):
        if isinstance(k, str):
            return k
        return self.names[id(k)]

    def _semof(self, src):
        if src in self.sem:
            return self.sem[src]
        return self.chan[src][0]

    def _need(self, e, ev, needs):
        if ev is None:
            return
        src, val = ev
        if src == 'pe' and e == 'pe':
            return
        if self.know[e].get(src, 0) >= val:
            return
        if needs.get(src, 0) < val:
            needs[src] = val

    def _collect(self, e, r, w):
        needs = {}
        for k in r:
            self._need(e, self.last_w.get(self.key(k)), needs)
        for k in w:
            kk = self.key(k)
            self._need(e, self.last_w.get(kk), needs)
            for src, val in self.readers.get(kk, {}).items():
                self._need(e, (src, val), needs)
        items = list(needs.items())
        for src, val in items:
            ck = self.clock.get((src, val), {})
            for s2, v2 in list(needs.items()):
                if s2 != src and ck.get(s2, 0) >= v2:
                    del needs[s2]
        for src, val in needs.items():
            self.eng[e].wait_ge(self._semof(src), val)
            self.n_wait += 1
            kn = self.know[e]
            if kn.get(src, 0) < val:
                kn[src] = val
            for s2, v2 in self.clock.get((src, val), {}).items():
                if kn.get(s2, 0) < v2:
                    kn[s2] = v2

    def _record(self, ev, r, w):
        for k in r:
            kk = self.key(k)
            d = self.readers.setdefault(kk, {})
            if d.get(ev[0], 0) < ev[1]:
                d[ev[0]] = ev[1]
        for k in w:
            kk = self.key(k)
            self.last_w[kk] = ev
            self.readers[kk] = {}

    cut = None

    def op(self, e, fn, r=(), w=(), dma=False, ch=None):
        if self.cut is not None and self.n_ins >= self.cut:
            return None
        if dma:
            return self._dma(e, fn, r, w, ch)
        self._collect(e, r, w)
        ins = fn(self.eng[e])
        self.cnt[e] += 1
        ins.then_inc(self.sem[e], 1)
        ev = (e, self.cnt[e])
        ck = dict(self.know[e])
        self.clock[ev] = ck
        self._record(ev, r, w)
        self.n_ins += 1
        return ev

    def _dma(self, q, fn, r, w, ch=None):
        self._collect(q, r, w)
        if ch is None:
            ch = self.key(w[0]) if len(w) else self.key(r[0])
        ch = "dma_" + ch + ("_sw" if q == 'pool' else "")
        if ch not in self.chan:
            self.chan[ch] = [self.nc.alloc_semaphore(ch), 0]
        ins = fn(self.eng[q])
        self.chan[ch][1] += 16
        ins.then_inc(self.chan[ch][0], 16)
        ev = (ch, self.chan[ch][1])
        self.clock[ev] = dict(self.know[q])
        self._record(ev, r, w)
        self.n_ins += 1
        return ev

    def dma(self, q, out, in_, r=(), w=(), ch=None, out_final=False):
        ev = self._dma(q, lambda e: e.dma_start(out=out, in_=in_), r, w, ch)
        if out_final:
            self.out_events.append(ev)
        return ev

    def finish(self):
        needs = {}
        for ev in self.out_events:
            if needs.get(ev[0], 0) < ev[1]:
                needs[ev[0]] = ev[1]
        for ch, (sem, count) in self.chan.items():
            if count > 0 and needs.get(ch, 0) < count:
                needs[ch] = count
        for src, val in needs.items():
            self.eng['sp'].wait_ge(self._semof(src), val)
        for e in ('pe', 'dve', 'act', 'pool'):
            if self.cnt[e] > 0:
                self.eng['sp'].wait_ge(self.sem[e], self.cnt[e])


def make_ident(c, identf, identb=None):
    n = 128
    nc = c.nc
    col = c.sb("mk_col", [n, n], F32)
    row = c.sb("mk_row", [n, 1], F32)
    c.op('pool', lambda e: e.iota(col[:], pattern=[[1, n]], base=0, channel_multiplier=0,
                                  allow_small_or_imprecise_dtypes=True), w=[col])
    c.op('pool', lambda e: e.iota(row[:], pattern=[[0, 1]], base=0, channel_multiplier=1,
                                  allow_small_or_imprecise_dtypes=True), w=[row])
    c.op('dve', lambda e: e.tensor_scalar(identf[:], col[:], row[:, 0:1], None, ALU.is_equal), r=[col, row], w=[identf])
    if identb is not None:
        c.op('dve', lambda e: e.tensor_copy(identb[:], identf[:]), r=[identf], w=[identb])


def _barrier(c):
    for e in ('pe', 'dve', 'act', 'pool', 'sp'):
        kn = c.know[e]
        for src in ('pe', 'dve', 'act', 'pool'):
            if c.cnt[src] > 0 and kn.get(src, 0) < c.cnt[src]:
                c.eng[e].wait_ge(c.sem[src], c.cnt[src])
                kn[src] = c.cnt[src]
        for ch, (sem, count) in c.chan.items():
            if count > 0 and kn.get(ch, 0) < count:
                c.eng[e].wait_ge(sem, count)
                kn[ch] = count


Ctx.barrier = _barrier

D_MODEL = 1024
NBLK_ALL = 64
NBLK_KVB = 34
EPS = 1e-6
NEG = -30000.0


class _Scope:
    def __init__(self, c, prefix):
        import contextlib
        self.c = c
        self.prefix = prefix
        self.stack = contextlib.ExitStack()

    def sb(self, name, shape, dt):
        t = self.stack.enter_context(self.c.nc.sbuf_tensor(self.prefix + name, list(shape), dt))
        self.c.names[id(t)] = self.prefix + name
        return t

    def ps(self, name, shape, dt=F32):
        t = self.stack.enter_context(self.c.nc.psum_tensor(self.prefix + name, list(shape), dt))
        self.c.names[id(t)] = self.prefix + name
        return t

    def close(self):
        self.stack.close()


def _bc(ap, axis, shape):
    return ap.unsqueeze(axis).to_broadcast(list(shape))


def build_nc(nown=32, nkv=NBLK_ALL, stop_after=None, dbg=0):
    nc = bass.Bass("TRN2", target_bir_lowering=False)
    DI = lambda name, shape, dt=F32: nc.dram_tensor(name, list(shape), dt, kind="ExternalInput").ap()
    xs = DI("xs", [8192, 1024])
    cT_d = DI("cT", [128, 8])
    w_ada = DI("w_ada", [1024, 6144])
    b_ada = DI("b_ada", [1, 6144])
    g1T_d = DI("g1T", [128, 8])
    g2row = DI("g2row", [1, 1024])
    gfrow = DI("gfrow", [1, 1024])
    w_in = DI("w_in", [1024, 2304])
    qg_d = DI("qg", [1, 64])
    kg_d = DI("kg", [1, 64])
    gnT_d = DI("gnT", [128, 8])
    w_out = DI("w_out", [1024, 1024])
    w_q = DI("w_q", [1024, 2048])
    k1T_d = DI("k1T", [128, 128])
    k2T_d = DI("k2T", [128, 128])
    peer_uv = DI("peer_uv", [16384, 2048])
    uvb = nc.dram_tensor("uvb_scr", [16384, 2048], BF16).ap()
    rope_d = DI("rope", [NBLK_ALL, 128, 2, 64])
    nbias_d = DI("nbias", [3, 128, 8, 640])
    out = nc.dram_tensor("out", [4096, 1024], F32, kind="ExternalOutput").ap()
    kbt_dram = nc.dram_tensor("kbt_scr", [NBLK_KVB, 128, 4, 128], BF16).ap()
    vb_dram = nc.dram_tensor("vb_scr", [NBLK_KVB, 128, 512], BF16).ap()

    c = Ctx(nc)
    op = c.op

    identf = c.sb("identf", [128, 128], F32)
    identb = c.sb("identb", [128, 128], BF16)
    make_ident(c, identf, identb)
    neghalf = c.sb("neghalf", [128, 8], F32)
    op('dve', lambda e: e.memset(neghalf[:], -0.5), w=[neghalf])
    iota16 = c.sb("iota16", [128, 16], F32)
    op('pool', lambda e: e.iota(iota16[:], pattern=[[1, 16]], base=0, channel_multiplier=0,
                                allow_small_or_imprecise_dtypes=True), w=[iota16])
    gate1_bc = c.sb("gate1_bc", [128, 1024], F32)
    gate2_bc = c.sb("gate2_bc", [128, 1024], F32)
    A2_bc = c.sb("A2_bc", [128, 1024], F32)
    B2_bc = c.sb("B2_bc", [128, 1024], F32)
    gf_bc = c.sb("gf_bc", [128, 1024], F32)
    modT = c.sb("modT", [128, 2, 8], F32)
    A1 = c.sb("A1", [128, 8], F32)
    g1T = c.sb("g1T_t", [128, 8], F32)
    gnT = c.sb("gnT_t", [128, 8], F32)
    qg = c.sb("qg_t", [128, 64], F32)
    kg = c.sb("kg_t", [128, 64], F32)
    negC = c.sb("negC", [128, 1], F32)
    k1T = c.sb("k1T_t", [128, 128], BF16)
    k2T = c.sb("k2T_t", [128, 128], BF16)

    c.dma('sp', g1T[:], g1T_d, w=[g1T])
    c.dma('sp', gnT[:], gnT_d, w=[gnT])
    c.dma('sp', qg[:], qg_d.to_broadcast([128, 64]), w=[qg])
    c.dma('sp', kg[:], kg_d.to_broadcast([128, 64]), w=[kg])
    c.dma('sp', gf_bc[:], gfrow.to_broadcast([128, 1024]), w=[gf_bc])
    c.dma('sp', A2_bc[:], g2row.to_broadcast([128, 1024]), w=[A2_bc])
    op('pool', lambda e: e.dma_start(out=k1T[:], in_=k1T_d), w=[k1T], dma=True)
    op('pool', lambda e: e.dma_start(out=k2T[:], in_=k2T_d), w=[k2T], dma=True)

    mq = c.sb("mq", [128, 2], F32)
    op('dve', lambda e: e.tensor_reduce(mq[:, 0:1], qg[:], AX.X, ALU.max, apply_absolute_value=True), r=[qg], w=[mq])
    op('dve', lambda e: e.tensor_reduce(mq[:, 1:2], kg[:], AX.X, ALU.max, apply_absolute_value=True), r=[kg, mq], w=[mq])
    op('dve', lambda e: e.scalar_tensor_tensor(negC[:], mq[:, 0:1], -8.0, mq[:, 1:2], ALU.mult, ALU.mult), r=[mq], w=[negC])

    S0 = _Scope(c, "s0_")
    cT = S0.sb("cT", [128, 8], F32)
    sc = S0.sb("sc", [128, 8], F32)
    scbc = S0.sb("scbc", [128, 8, 128], F32)
    wada = [S0.sb("wada%d" % i, [128, 8, 512], F32) for i in range(2)]
    bb = [S0.sb("bb%d" % i, [128, 512], F32) for i in range(2)]
    modg = [S0.sb("modg%d" % i, [128, 512], F32) for i in range(2)]
    psm = [S0.ps("psm%d" % i, [128, 512], F32) for i in range(2)]
    pst = S0.ps("pst", [128, 4, 128], F32)
    c.dma('sp', cT[:], cT_d, w=[cT])
    op('act', lambda e: e.activation(sc[:], cT[:], AF.Silu), r=[cT], w=[sc])
    op('dve', lambda e: e.tensor_copy(scbc[:], _bc(sc[:], 2, [128, 8, 128])), r=[sc], w=[scbc])
    w_ada_v = w_ada.rearrange("(j p) n -> p j n", p=128)
    for gi in range(12):
        wt = wada[gi % 2]; bt = bb[gi % 2]; mg = modg[gi % 2]; pm = psm[gi % 2]
        c.dma('sp', wt[:], w_ada_v[:, :, gi * 512:(gi + 1) * 512], w=[wt])
        c.dma('sp', bt[:], b_ada[0:1, gi * 512:(gi + 1) * 512].to_broadcast([128, 512]), w=[bt])
        for j in range(8):
            op('pe', lambda e: e.matmul(pm[:], scbc[:, j, :], wt[:, j, :], start=(j == 0), stop=(j == 7)),
               r=[scbc, wt], w=[pm])
        piece, half = gi // 2, gi % 2
        hs = slice(half * 512, (half + 1) * 512)
        if piece in (0, 1):
            op('dve', lambda e: e.tensor_tensor(mg[:], pm[:], bt[:], ALU.add), r=[pm, bt], w=[mg])
            for k in range(4):
                op('pe', lambda e: e.transpose(pst[:, k, :], mg[:, k * 128:(k + 1) * 128], identf[:]),
                   r=[mg, identf], w=[pst])
            op('dve', lambda e: e.tensor_copy(modT[:, piece, half * 4:(half + 1) * 4], pst[:, :, 0]), r=[pst], w=[modT])
        else:
            dst = {2: gate1_bc, 3: B2_bc, 4: None, 5: gate2_bc}[piece]
            if dst is not None:
                op('dve', lambda e: e.tensor_tensor(dst[:, hs], pm[:], bt[:], ALU.add), r=[pm, bt], w=[dst])
            else:
                op('dve', lambda e: e.tensor_tensor(mg[:], pm[:], bt[:], ALU.add), r=[pm, bt], w=[mg])
                op('dve', lambda e: e.scalar_tensor_tensor(A2_bc[:, hs], mg[:], 1.0, A2_bc[:, hs], ALU.add, ALU.mult),
                   r=[mg, A2_bc], w=[A2_bc])
    op('dve', lambda e: e.scalar_tensor_tensor(A1[:], modT[:, 1, :], 1.0, g1T[:], ALU.add, ALU.mult), r=[modT, g1T], w=[A1])
    c.barrier()
    S0.close()
    if stop_after == 'S0':
        c.dma('sp', out[0:128, :], gate1_bc[:], r=[gate1_bc], out_final=True)
        c.dma('sp', out[128:256, :], A2_bc[:], r=[A2_bc], out_final=True)
        c.dma('sp', out[256:384, 0:8], A1[:], r=[A1], out_final=True)
        c.dma('sp', out[256:384, 8:24], modT[:, :, :].rearrange("p a b -> p (a b)"), r=[modT], out_final=True)
        c.finish()
        return nc

    S1 = _Scope(c, "s1_")
    Win = S1.sb("Win", [128, 8, 2304], BF16)
    Wout = S1.sb("Wout", [128, 8, 1024], BF16)
    KAT = S1.sb("KAT", [128, 8192], BF16)
    VA = S1.sb("VA", [128, NBLK_ALL, 2, 65], BF16)
    xbuf = [S1.sb("x%d" % i, [128, 1024], F32) for i in range(2)]
    ropeb = [S1.sb("rope%d" % i, [128, 2, 64], F32) for i in range(2)]
    xn = S1.sb("xn", [128, 1024], BF16)
    hT = S1.sb("hT", [128, 8, 128], BF16)
    st1 = S1.sb("st1", [128, 16], F32)
    D = [S1.ps("D%d" % i, [128, 1024], F32) for i in range(4)]
    Dk = lambda i, h: "D%d%s" % (i, "ab"[h])

    w_in_v = w_in.rearrange("(j p) n -> p j n", p=128)
    for k in range(3):
        op('pool', lambda e: e.dma_start(out=Win[:, :, k * 768:(k + 1) * 768], in_=w_in_v[:, :, k * 768:(k + 1) * 768]),
           w=[Win], dma=True)
    op('pool', lambda e: e.dma_start(out=Wout[:], in_=w_out.rearrange("(j p) n -> p j n", p=128)), w=[Wout], dma=True)
    op('dve', lambda e: e.memset(VA[:, :, :, 64:65], 1.0), w=[VA])

    A1b = _bc(A1[:], 2, [128, 8, 128])
    B1b = _bc(modT[:, 0, :], 2, [128, 8, 128])
    pTv = D[0][:, 0:512].bitcast(BF16).rearrange("p (j n) -> p j n", j=8)

    def load_x(i):
        c.dma('sp', xbuf[i % 2][:], xs[i * 128:(i + 1) * 128, :], w=[xbuf[i % 2]])
        c.dma('sp', ropeb[i % 2][:], rope_d[i], w=[ropeb[i % 2]])

    def norm_hT_g(xt):
        op('act', lambda e: e.activation(xn[:], xt[:], AF.Square, accum_out=st1[:, 0:1]), r=[xt], w=[st1, xn]); yield
        op('dve', lambda e: e.tensor_scalar(st1[:, 1:2], st1[:, 0:1], 1.0 / D_MODEL, EPS, ALU.mult, ALU.add), r=[st1], w=[st1]); yield
        op('act', lambda e: e.activation(st1[:, 3:4], st1[:, 1:2], AF.Ln), r=[st1], w=[st1]); yield
        op('act', lambda e: e.activation(st1[:, 2:3], st1[:, 3:4], AF.Exp, scale=-0.5), r=[st1], w=[st1]); yield
        op('act', lambda e: e.activation(xn[:], xt[:], AF.Identity, scale=st1[:, 2:3]), r=[xt, st1], w=[xn]); yield
        for j in range(8):
            op('pe', lambda e: e.transpose(pTv[:, j, :], xn[:, j * 128:(j + 1) * 128], identb[:]), r=[xn, identb], w=["D0a"])
        yield
        op('dve', lambda e: e.tensor_tensor(hT[:], pTv, A1b, ALU.mult), r=["D0a", A1], w=[hT]); yield
        op('dve', lambda e: e.tensor_tensor(hT[:], hT[:], B1b, ALU.add), r=[hT, modT], w=[hT]); yield

    def norm_hT(xt):
        for _ in norm_hT_g(xt):
            pass

    def head_norm_rope_g(dst_bf, src_sb, H, gain, rp, scr):
        sq, ssq, tmp, t1, t2 = scr
        sv = src_sb[:, 0:H * 64].rearrange("p (h d) -> p h d", h=H)
        sqv = sq[:, 0:H * 64].rearrange("p (h d) -> p h d", h=H)
        op('dve', lambda e: e.tensor_tensor(sqv, sv, sv, ALU.mult), r=[src_sb], w=[sq])
        yield
        op('dve', lambda e: e.tensor_reduce(ssq[:, 0:H], sqv, AX.X, ALU.add), r=[sq], w=[ssq])
        yield
        op('dve', lambda e: e.tensor_scalar(ssq[:, 8:8 + H], ssq[:, 0:H], 1.0 / 64, EPS, ALU.mult, ALU.add), r=[ssq], w=[ssq])
        yield
        op('act', lambda e: e.activation(ssq[:, 0:H], ssq[:, 8:8 + H], AF.Ln), r=[ssq], w=[ssq])
        yield
        op('act', lambda e: e.activation(ssq[:, 16:16 + H], ssq[:, 0:H], AF.Exp, scale=-0.5), r=[ssq], w=[ssq])
        yield
        tv = tmp[:, 0:H * 64].rearrange("p (h d) -> p h d", h=H)
        op('dve', lambda e: e.tensor_tensor(tv, sv, _bc(ssq[:, 16:16 + H], 2, [128, H, 64]), ALU.mult), r=[src_sb, ssq], w=[tmp])
        yield
        op('dve', lambda e: e.tensor_tensor(tv, tv, _bc(gain[:], 1, [128, H, 64]), ALU.mult), r=[tmp, gain], w=[tmp])
        yield
        t1v = t1[:, 0:H * 64].rearrange("p (h d) -> p h d", h=H)
        op('dve', lambda e: e.tensor_tensor(t1v, tv, _bc(rp[:, 0, :], 1, [128, H, 64]), ALU.mult), r=[tmp, rp], w=[t1])
        yield
        x5 = tmp[:, 0:H * 64].rearrange("p (h a b d) -> p h a b d", h=H, a=2, b=2)
        o5 = t2[:, 0:H * 64].rearrange("p (h a b d) -> p h a b d", h=H, a=2, b=2)
        s5 = rp[:, 1, :].rearrange("p (a b d) -> p a b d", a=2, b=2)
        for b_ in range(2):
            op('dve', lambda e: e.tensor_tensor(o5[:, :, :, b_, :], x5[:, :, :, 1 - b_, :],
                                                _bc(s5[:, :, b_, :], 1, [128, H, 2, 16]), ALU.mult), r=[tmp, rp], w=[t2])
            yield
        op('dve', lambda e: e.tensor_tensor(dst_bf[:, 0:H * 64], t1[:, 0:H * 64], t2[:, 0:H * 64], ALU.add), r=[t1, t2], w=[dst_bf])
        yield


    def head_norm_rope(dst_bf, src_sb, H, gain, rp, scr):
        for _ in head_norm_rope_g(dst_bf, src_sb, H, gain, rp, scr):
            pass

    ka_sb = S1.sb("ka_sb", [128, 128], F32)
    scrA = (S1.sb("sqA", [128, 512], F32), S1.sb("ssqA", [128, 24], F32), S1.sb("tmpA", [128, 512], F32),
            S1.sb("t1A", [128, 512], F32), S1.sb("t2A", [128, 512], F32))
    kr = S1.sb("kr", [128, 128], BF16)
    kbs = [S1.sb("kbs%d" % i, [128, 4, 128], BF16) for i in range(2)]
    vbs = [S1.sb("vbs%d" % i, [128, 512], BF16) for i in range(2)]
    pA = D[0][:, 512:768]
    pKv = D[1][:, 0:64].bitcast(BF16)
    pKB = D[2][:, 0:512].rearrange("p (f n) -> p f n", f=4)
    pVB = D[3][:, 0:512]
    cvb = [S1.sb("cv%d" % i, [128, 2048], BF16) for i in range(2)]
    n_cv = 128
    per_blk = -(-n_cv // nkv)

    def convert(k):
        cv = cvb[k % 2]
        op('pool', lambda e: e.dma_start(out=cv[:], in_=peer_uv[k * 128:(k + 1) * 128, :]), w=[cv], dma=True)
        c.dma('sp', uvb[k * 128:(k + 1) * 128, :], cv[:], r=[cv])
    load_x(0)
    for i in range(nkv):
        if i + 1 < nkv:
            load_x(i + 1)
        for k in range(i * per_blk, min((i + 1) * per_blk, n_cv)):
            convert(k)
        xt = xbuf[i % 2]; rp = ropeb[i % 2]
        norm_hT(xt)
        for j in range(8):
            op('pe', lambda e: e.matmul(pA, hT[:, j, :], Win[:, j, 512:768], start=(j == 0), stop=(j == 7)), r=[hT, Win], w=["D0b"])
        op('act', lambda e: e.copy(ka_sb[:], pA[:, 0:128]), r=["D0b"], w=[ka_sb])
        op('act', lambda e: e.copy(VA[:, i, :, 0:64], pA[:, 128:256].rearrange("p (g d) -> p g d", g=2)), r=["D0b"], w=[VA])
        head_norm_rope(kr, ka_sb, 2, kg, rp, scrA)
        op('pe', lambda e: e.transpose(pKv, kr[:], identb[:]), r=[kr, identb], w=["D1a"])
        op('act', lambda e: e.copy(KAT[:, i * 128:(i + 1) * 128], pKv), r=["D1a"], w=[KAT])
        if i < NBLK_KVB:
            for f in range(4):
                for j in range(8):
                    op('pe', lambda e: e.matmul(pKB[:, f, :], Win[:, j, 1280 + f * 128:1280 + (f + 1) * 128], hT[:, j, :],
                                                start=(j == 0), stop=(j == 7)), r=[hT, Win], w=["D2a"])
            ks = kbs[i % 2]; vs = vbs[i % 2]
            op('act', lambda e: e.copy(ks[:], pKB), r=["D2a"], w=[ks])
            c.dma('sp', kbt_dram[i], ks[:], r=[ks])
            for j in range(8):
                op('pe', lambda e: e.matmul(pVB, hT[:, j, :], Win[:, j, 1792:2304], start=(j == 0), stop=(j == 7)), r=[hT, Win], w=["D3a"])
            op('dve', lambda e: e.tensor_copy(vs[:], pVB), r=["D3a"], w=[vs])
            c.dma('sp', vb_dram[i], vs[:], r=[vs])
    c.barrier()
    if stop_after == 'A':
        dbg = S1.sb("dbg", [128, 1024], F32)
        c.op('dve', lambda e: e.tensor_copy(dbg[:, 0:256], KAT[:, 0:256]), r=[KAT], w=[dbg])
        c.op('dve', lambda e: e.tensor_copy(dbg[:, 256:516], VA[:, 0:2, :, :].rearrange("p a g d -> p (a g d)")), r=[VA], w=[dbg])
        c.dma('sp', out[0:128, :], dbg[:], r=[dbg], out_final=True)
        dbg2 = S1.sb("dbg2", [128, 1024], BF16)
        c.dma('sp', dbg2[:, 0:512], kbt_dram[1].rearrange("p f t -> p (f t)"), w=[dbg2])
        c.dma('sp', dbg2[:, 512:1024], vb_dram[1], w=[dbg2])
        dbg3 = S1.sb("dbg3", [128, 1024], F32)
        c.op('dve', lambda e: e.tensor_copy(dbg3[:], dbg2[:]), r=[dbg2], w=[dbg3])
        c.dma('sp', out[128:256, :], dbg3[:], r=[dbg3], out_final=True)
        c.finish()
        return nc

    kwin = [S1.sb("kwin%d" % i, [128, 5, 4, 128], BF16) for i in range(1)]
    vwin = [S1.sb("vwin%d" % i, [128, 5, 512], BF16) for i in range(1)]
    nbt = S1.sb("nbt", [128, 8, 640], F32)
    qa_sb = S1.sb("qa_sb", [128, 512], F32)
    qr = S1.sb("qr", [128, 512], BF16)
    qb = S1.sb("qb", [128, 512], BF16)
    qAT = [S1.sb("qAT%d" % i, [128, 4, 128], BF16) for i in range(2)]
    qBTs = [S1.sb("qBT%d" % i, [128, 4, 128], BF16) for i in range(2)]
    PT2 = [S1.sb("PT2_%d" % i, [128, 1024], BF16) for i in range(3)]
    oT = S1.sb("oT", [65, 2, 512], F32)
    st2 = S1.sb("st2", [128, 48], F32)
    mixf = S1.sb("mixf", [128, 1024], F32)
    mixn = S1.sb("mixn", [128, 1024], BF16)
    mixT = S1.sb("mixT", [128, 8, 128], BF16)
    s_sb = S1.sb("s_sb", [128, 640], F32)
    p_sb = S1.sb("p_sb", [128, 640], BF16)
    PTn = S1.sb("PTn", [128, 5, 128], BF16)
    x1t = [S1.sb("x1t%d" % i, [128, 1024], F32) for i in range(1)]

    def win_start(j):
        return min(max(2 * j - 4, 0), 58) // 2

    def load_win(j):
        cb = win_start(j)
        c.dma('sp', kwin[0][:], kbt_dram[cb:cb + 5].rearrange("b p f t -> p b f t"), w=[kwin[0]])
        c.dma('sp', vwin[0][:], vb_dram[cb:cb + 5].rearrange("b p n -> p b n"), w=[vwin[0]])

    Dp = [D[1], D[2], D[3]]
    Dpk = [("D1a", "D1b"), ("D2a", "D2b"), ("D3a", "D3b")]
    pO = [D[0][0:65, 0:512], D[0][0:65, 512:1024]]
    pOk = ["D0a", "D0b"]
    pT2 = D[0][:, 512:1024].bitcast(BF16).rearrange("p (j n) -> p j n", j=8)
    pTa = D[1][:, :].rearrange("p (s n) -> p s n", s=8)
    import os
    print("B1 start n_ins", c.n_ins)
    if os.environ.get("CUT"):
        c.cut = c.n_ins + int(os.environ["CUT"])
    for g_ in range(2):
        op('dve', lambda e: e.memset(qAT[g_][:], 0.0), w=[qAT[g_]])
    def pre_gen(jb):
        xt_ = xbuf[jb % 2]; rp_ = ropeb[jb % 2]; qBT_ = qBTs[jb % 2]
        yield from norm_hT_g(xt_)
        for hh, c0 in ((0, 0), (1, 768)):
            for jj in range(8):
                op('pe', lambda e: e.matmul(D[3][:, hh * 512:(hh + 1) * 512], hT[:, jj, :], Win[:, jj, c0:c0 + 512],
                                            start=(jj == 0), stop=(jj == 7)), r=[hT, Win], w=[Dk(3, hh)])
            yield
        op('act', lambda e: e.copy(qa_sb[:], D[3][:, 0:512]), r=["D3a"], w=[qa_sb]); yield
        op('act', lambda e: e.copy(qb[:], D[3][:, 512:1024]), r=["D3b"], w=[qb]); yield
        yield from head_norm_rope_g(qr, qa_sb, 8, qg, rp_, scrA)
        for k in range(4):
            op('pe', lambda e: e.transpose(pT2[:, k, :], qr[:, k * 128:(k + 1) * 128], identb[:]), r=[qr, identb], w=["D0b"])
        for k in range(4):
            op('pe', lambda e: e.transpose(pT2[:, 4 + k, :], qb[:, k * 128:(k + 1) * 128], identb[:]), r=[qb, identb], w=["D0b"])
        yield
        for g_ in range(2):
            op('dve', lambda e: e.tensor_copy(qAT[g_][64 * g_:64 * g_ + 64, :, :], pT2[64 * g_:64 * g_ + 64, 0:4, :]), r=["D0b"], w=[qAT[g_]])
            yield
        op('dve', lambda e: e.tensor_copy(qBT_[:], pT2[:, 4:8, :]), r=["D0b"], w=[qBT_]); yield

    def _drain(g):
        if g is not None:
            for _ in g:
                pass

    def _step(g, n):
        if g is not None:
            for _ in range(n):
                try:
                    next(g)
                except StopIteration:
                    return

    if nown > 0:
        load_x(0)
        if not os.environ.get("NO_WIN"):
            load_win(0)
        _drain(pre_gen(0))
    for j in range(nown):
        xt = xbuf[j % 2]; rp = ropeb[j % 2]; qBT = qBTs[j % 2]
        if j + 1 < nown:
            load_x(j + 1)
        if j <= 2 and not os.environ.get("NO_NBT"):
            c.dma('sp', nbt[:], nbias_d[j], w=[nbt])
        for g in range(2):
            pb = 64 * g
            npair = nkv // 2

            def S2(pi):
                for u in range(2):
                    kc = 2 * pi + u
                    op('pe', lambda e: e.matmul(Dp[pi % 3][:, u * 512:(u + 1) * 512], KAT[:, kc * 128:(kc + 1) * 128],
                                                qAT[g][:, :, :], start=True, stop=True), r=[KAT, qAT[g]], w=[Dpk[pi % 3][u]])

            def EXP2(pi):
                op('act', lambda e: e.activation(PT2[pi % 3][:], Dp[pi % 3][:, :], AF.Exp, scale=0.125, bias=negC[:, 0:1]),
                   r=[Dpk[pi % 3][0], Dpk[pi % 3][1], negC], w=[PT2[pi % 3]])

            def PV2(pi):
                for u in range(2):
                    kc = 2 * pi + u
                    op('pe', lambda e: e.matmul(pO[g], VA[:, kc, g, :], PT2[pi % 3][:, u * 512:(u + 1) * 512],
                                                start=(kc == 0), stop=(kc == nkv - 1)), r=[VA, PT2[pi % 3]], w=[pOk[g]])
            S2(0)
            if npair > 1:
                S2(1)
            for pi in range(npair):
                EXP2(pi)
                if pi + 2 < npair:
                    S2(pi + 2)
                PV2(pi)
            op('dve', lambda e: e.tensor_copy(oT[:, g, :], pO[g]), r=[pOk[g]], w=[oT])
        for g in range(2):
            for i in range(4):
                op('pe', lambda e: e.transpose(pTa[:, g * 4 + i, 0:65], oT[0:65, g, i * 128:(i + 1) * 128], identf[0:65, 0:65]),
                   r=[oT, identf], w=["D1a", "D1b"])
        op('dve', lambda e: e.reciprocal(st2[:, 0:8], pTa[:, :, 64]), r=["D1a", "D1b"], w=[st2])
        op('dve', lambda e: e.tensor_tensor(mixf[:, 0:512].rearrange("p (s d) -> p s d", s=8), pTa[:, :, 0:64],
                                            _bc(st2[:, 0:8], 2, [128, 8, 64]), ALU.mult), r=["D1a", "D1b", st2], w=[mixf])
        if dbg == 2:
            c.dma('sp', out[0:128, :], mixf[:], r=[mixf], out_final=True)
            c.finish()
            return nc
        kw = kwin[0]; vw = vwin[0]
        pNs = [D[2], D[2]]
        pNk = [("D2a", "D2b"), ("D2a", "D2b")]
        nxt_pre = pre_gen(j + 1) if j + 1 < nown else None
        pPT = D[1][:, 0:320].bitcast(BF16).rearrange("p (c n) -> p c n", c=5)
        pOB = D[1][:, 512:1024]
        s_sbs = [s_sb[:], cvb[0][:, :].bitcast(F32)[:, 0:640]]
        s_sbk = [s_sb, cvb[0]]
        p_sbs = [p_sb[:], cvb[1][:, 0:640]]
        p_sbk = [p_sb, cvb[1]]

        def na_front(h):
            pr, hb = h // 2, 64 * (h % 2)
            pN = pNs[h % 2]; ss_ = s_sbs[h % 2]; ps_ = p_sbs[h % 2]
            op('pe', lambda e: e.matmul(pN[:, 0:512], qBT[hb:hb + 64, pr, :], kw[hb:hb + 64, 0:4, pr, :], start=True, stop=True),
               r=[qBT, kw], w=[pNk[h % 2][0]])
            op('pe', lambda e: e.matmul(pN[:, 512:640], qBT[hb:hb + 64, pr, :], kw[hb:hb + 64, 4, pr, :], start=True, stop=True),
               r=[qBT, kw], w=[pNk[h % 2][1]])
            op('dve', lambda e: e.scalar_tensor_tensor(ss_, pN[:, 0:640], 0.125, nbt[:, h, :], ALU.mult, ALU.add),
               r=[pNk[h % 2][0], pNk[h % 2][1], nbt], w=[s_sbk[h % 2]])
            op('dve', lambda e: e.tensor_reduce(st2[:, 8 + h % 2:9 + h % 2], ss_, AX.X, ALU.max, negate=True), r=[s_sbk[h % 2]], w=["st2_nm%d" % (h % 2)])
            op('act', lambda e: e.activation(ps_, ss_, AF.Exp, bias=st2[:, 8 + h % 2:9 + h % 2], accum_out=st2[:, 16 + h:17 + h]),
               r=[s_sbk[h % 2], "st2_nm%d" % (h % 2)], w=[p_sbk[h % 2], "st2_rs%d" % h])
        na_front(0)
        for h in range(8):
            if h + 1 < 8:
                na_front(h + 1)
            ps_ = p_sbs[h % 2]
            for cc in range(5):
                op('pe', lambda e: e.transpose(pPT[:, cc, :], ps_[:, cc * 128:(cc + 1) * 128], identb[:]), r=[p_sbk[h % 2], identb], w=["D1a"])
            op('dve', lambda e: e.tensor_copy(PTn[:], pPT), r=["D1a"], w=[PTn])
            for cc in range(5):
                op('pe', lambda e: e.matmul(pOB[:, h * 64:(h + 1) * 64], PTn[:, cc, :], vw[:, cc, h * 64:(h + 1) * 64],
                                            start=(cc == 0), stop=(cc == 4)), r=[PTn, vw], w=["D1b"])
            _step(nxt_pre, 4)
        _drain(nxt_pre)
        if j + 1 < nown:
            load_win(j + 1)
        op('dve', lambda e: e.reciprocal(st2[:, 24:32], st2[:, 16:24]), r=["st2_rs%d" % h_ for h_ in range(8)], w=["st2_ri"])
        op('dve', lambda e: e.tensor_tensor(mixf[:, 512:1024].rearrange("p (s d) -> p s d", s=8),
                                            pOB.rearrange("p (s d) -> p s d", s=8),
                                            _bc(st2[:, 24:32], 2, [128, 8, 64]), ALU.mult), r=["D1b", "st2_ri"], w=[mixf])
        if dbg == 3:
            c.dma('sp', out[0:128, :], mixf[:], r=[mixf], out_final=True)
            c.finish()
            return nc
        for gi in range(2):
            op('dve', lambda e: e.scalar_tensor_tensor(s_sb[:, 0:512], mixf[:, gi * 512:(gi + 1) * 512], 1.0, mixf[:, gi * 512:(gi + 1) * 512],
                                                       ALU.mult, ALU.mult, accum_out=st2[:, 32 + gi:33 + gi]), r=[mixf], w=[st2, s_sb])
        op('dve', lambda e: e.tensor_scalar(st2[:, 34:36], st2[:, 32:34], 1.0 / 512, EPS, ALU.mult, ALU.add), r=[st2], w=[st2])
        op('act', lambda e: e.activation(st2[:, 38:40], st2[:, 34:36], AF.Ln), r=[st2], w=[st2])
        op('act', lambda e: e.activation(st2[:, 36:38], st2[:, 38:40], AF.Exp, scale=-0.5), r=[st2], w=[st2])
        for gi in range(2):
            op('act', lambda e: e.activation(mixn[:, gi * 512:(gi + 1) * 512], mixf[:, gi * 512:(gi + 1) * 512], AF.Identity,
                                             scale=st2[:, 36 + gi:37 + gi]), r=[mixf, st2], w=[mixn])
        for k in range(8):
            op('pe', lambda e: e.transpose(pTv[:, k, :], mixn[:, k * 128:(k + 1) * 128], identb[:]), r=[mixn, identb], w=["D0a"])
        op('dve', lambda e: e.tensor_tensor(mixT[:], pTv, _bc(gnT[:], 2, [128, 8, 128]), ALU.mult), r=["D0a", gnT], w=[mixT])
        for hh in range(2):
            for jj in range(8):
                op('pe', lambda e: e.matmul(D[1][:, hh * 512:(hh + 1) * 512], mixT[:, jj, :], Wout[:, jj, hh * 512:(hh + 1) * 512],
                                            start=(jj == 0), stop=(jj == 7)), r=[mixT, Wout], w=[Dk(1, hh)])
        xo = x1t[0]
        op('dve', lambda e: e.tensor_tensor(mixf[:], D[1][:, :], gate1_bc[:], ALU.mult), r=["D1a", "D1b", gate1_bc], w=[mixf])
        op('dve', lambda e: e.tensor_tensor(xo[:], mixf[:], xt[:], ALU.add), r=[mixf, xt], w=[xo])
        c.dma('sp', out[j * 128:(j + 1) * 128, :], xo[:], r=[xo], out_final=True)
    c.barrier()
    S1.close()
    if stop_after == 'B1':
        c.finish()
        return nc
    _phase_c(c, nc, nown, out, w_q, uvb, k1T, k2T, identb, neghalf, iota16, gate2_bc, A2_bc, B2_bc, gf_bc)
    c.finish()
    return nc


NB = 22
GS = 4
LEAD = 10


def _phase_c(c, nc, nown, out, w_q, uvb, k1T, k2T, identb, neghalf, iota16, gate2_bc, A2_bc, B2_bc, gf_bc, pipeline=True):
    op = c.op
    S2 = _Scope(c, "s2_")
    Wq = S2.sb("Wq", [128, 8, 2048], BF16)
    w_q_v = w_q.rearrange("(j p) n -> p j n", p=128)
    for k in range(2):
        op('pool', lambda e: e.dma_start(out=Wq[:, :, k * 1024:(k + 1) * 1024], in_=w_q_v[:, :, k * 1024:(k + 1) * 1024]),
           w=[Wq], dma=True)
    x1b = [S2.sb("x1b%d" % i, [128, 1024], F32) for i in range(2)]
    h2s = [S2.sb("h2_%d" % i, [128, 1024], F32) for i in range(1)]
    h2bs = [S2.sb("h2b%d" % i, [128, 1024], BF16) for i in range(2)]
    prod = [S2.sb("prod%d" % i, [128, 1024], BF16) for i in range(4)]
    eidxs = [S2.sb("eidx%d" % i, [128, 128], I32) for i in range(2)]
    gws = [S2.sb("gw%d" % i, [128, 8, 16], F32) for i in range(2)]
    st = S2.sb("st", [128, 16], F32)
    stt = S2.sb("stt", [128, 16], F32)
    xn2 = S2.sb("xn2", [128, 1024], F32)
    h2T = S2.sb("h2T", [128, 8, 128], BF16)
    qT = S2.sb("qT", [128, 16, 128], BF16)
    s_sb = S2.sb("s_sb", [128, 16, 128], F32)
    wk = S2.sb("wk", [128, 256], F32)
    vals = S2.sb("vals", [128, 16, 16], F32)
    idxs = S2.sb("idxs", [128, 16, 16], U32)
    idxf = S2.sb("idxf", [128, 16, 16], F32)
    cand = S2.sb("cand", [128, 8, 256], F32)
    eq = cand
    eqv = cand[:, :, :].rearrange("p h (k m) -> p h k m", k=16)
    tv = S2.sb("tv", [128, 8, 16], F32)
    tp = S2.sb("tp", [128, 8, 16], U32)
    ij = S2.sb("ij", [128, 2, 128], U32)
    ijf = S2.sb("ijf", [128, 2, 128], F32)
    I12 = S2.sb("I12", [128, 2, 128], F32)
    eidf = S2.sb("eidf", [128, 128], F32)
    sm = S2.sb("sm", [128, 16], F32)
    a_t = S2.sb("a_t", [128, 128], F32)
    ga = S2.sb("ga", [128, 128], F32)
    wgt = S2.sb("wgt", [128, 128], F32)
    gb = [S2.sb("gb%d" % i, [128, 2048], BF16) for i in range(NB)]
    dg = [S2.sb("dg%d" % i, [128, 128], BF16) for i in range(4)]
    yt = h2s[0]
    xo = xn2
    D = [S2.ps("D%d" % i, [128, 1024], F32) for i in range(4)]
    pTv = D[0][:, 0:512].bitcast(BF16).rearrange("p (j n) -> p j n", j=8)
    pacc = D[3]

    def load(j):
        c.dma('sp', x1b[j % 2][:], out[j * 128:(j + 1) * 128, :], w=[x1b[j % 2]])

    def front(j):
        x1 = x1b[j % 2]; h2 = h2s[0]; h2b = h2bs[j % 2]; eidx = eidxs[j % 2]; gw = gws[j % 2]
        op('act', lambda e: e.activation(xn2[:], x1[:], AF.Square, accum_out=st[:, 0:1]), r=[x1], w=[st, xn2]); yield
        op('dve', lambda e: e.tensor_scalar(st[:, 1:2], st[:, 0:1], 1.0 / D_MODEL, EPS, ALU.mult, ALU.add), r=[st], w=[st]); yield
        op('act', lambda e: e.activation(st[:, 3:4], st[:, 1:2], AF.Ln), r=[st], w=[st])
        op('act', lambda e: e.activation(st[:, 2:3], st[:, 3:4], AF.Exp, scale=-0.5), r=[st], w=[st])
        op('act', lambda e: e.activation(xn2[:], x1[:], AF.Identity, scale=st[:, 2:3]), r=[x1, st], w=[xn2]); yield
        op('dve', lambda e: e.tensor_tensor(h2[:], xn2[:], A2_bc[:], ALU.mult), r=[xn2, A2_bc], w=[h2]); yield
        op('dve', lambda e: e.tensor_tensor(h2[:], h2[:], B2_bc[:], ALU.add), r=[h2, B2_bc], w=[h2]); yield
        op('act', lambda e: e.copy(h2b[:], h2[:]), r=[h2], w=[h2b])
        for k in range(8):
            op('pe', lambda e: e.transpose(pTv[:, k, :], h2b[:, k * 128:(k + 1) * 128], identb[:]), r=[h2b, identb], w=["E0a"])
        op('dve', lambda e: e.tensor_copy(h2T[:], pTv), r=["E0a"], w=[h2T]); yield
        for hf in range(2):
            dq = D[2][:, :].rearrange("p (s n) -> p s n", s=8)
            for f8 in range(8):
                f = hf * 8 + f8
                for jj in range(8):
                    op('pe', lambda e: e.matmul(dq[:, f8, :], Wq[:, jj, f * 128:(f + 1) * 128], h2T[:, jj, :],
                                                start=(jj == 0), stop=(jj == 7)), r=[Wq, h2T], w=["E2"])
            op('act', lambda e: e.copy(qT[:, hf * 8:(hf + 1) * 8, :], dq), r=["E2"], w=[qT]); yield
        for hf in range(2):
            ds = D[1][:, :].rearrange("p (s n) -> p s n", s=8)
            for f8 in range(8):
                f = hf * 8 + f8
                kk = k1T if f % 2 == 0 else k2T
                op('pe', lambda e: e.matmul(ds[:, f8, :], qT[:, f, :], kk[:], start=True, stop=True), r=[qT, kk], w=["E1"])
            op('act', lambda e: e.copy(s_sb[:, hf * 8:(hf + 1) * 8, :], ds), r=["E1"], w=[s_sb]); yield
        for f in range(16):
            op('dve', lambda e: e.max(vals[:, f, 0:8], s_sb[:, f, :]), r=[s_sb], w=[vals]); yield
            op('dve', lambda e: e.max_index(idxs[:, f, 0:8], vals[:, f, 0:8], s_sb[:, f, :]), r=[s_sb, vals], w=[idxs]); yield
            op('dve', lambda e: e.match_replace(wk[:, 0:128], vals[:, f, 0:8], s_sb[:, f, :], -1e30), r=[s_sb, vals], w=[wk]); yield
            op('dve', lambda e: e.max(vals[:, f, 8:16], wk[:, 0:128]), r=[wk], w=[vals]); yield
            op('dve', lambda e: e.max_index(idxs[:, f, 8:16], vals[:, f, 8:16], wk[:, 0:128]), r=[wk, vals], w=[idxs]); yield
        v4 = vals[:, :, :].rearrange("p (h a) k -> p h a k", a=2)
        op('dve', lambda e: e.tensor_tensor(cand[:, :, :].rearrange("p h (i k) -> p h i k", i=16),
                                            _bc(v4[:, :, 0, :], 3, [128, 8, 16, 16]), _bc(v4[:, :, 1, :], 2, [128, 8, 16, 16]), ALU.add),
           r=[vals], w=[cand]); yield
        for h in range(8):
            op('dve', lambda e: e.max(tv[:, h, 0:8], cand[:, h, :]), r=[cand], w=[tv]); yield
            op('dve', lambda e: e.max_index(tp[:, h, 0:8], tv[:, h, 0:8], cand[:, h, :]), r=[cand, tv], w=[tp]); yield
            op('dve', lambda e: e.match_replace(wk[:], tv[:, h, 0:8], cand[:, h, :], -1e30), r=[cand, tv], w=[wk]); yield
            op('dve', lambda e: e.max(tv[:, h, 8:16], wk[:]), r=[wk], w=[tv]); yield
            op('dve', lambda e: e.max_index(tp[:, h, 8:16], tv[:, h, 8:16], wk[:]), r=[wk, tv], w=[tp]); yield
        tpf = tp[:, :, :].rearrange("p h k -> p (h k)")
        op('dve', lambda e: e.tensor_single_scalar(ij[:, 0, :], tpf, 4, ALU.logical_shift_right), r=[tp], w=[ij]); yield
        op('dve', lambda e: e.tensor_single_scalar(ij[:, 1, :], tpf, 15, ALU.bitwise_and), r=[tp, ij], w=[ij]); yield
        op('dve', lambda e: e.tensor_copy(ijf[:], ij[:]), r=[ij], w=[ijf]); yield
        op('dve', lambda e: e.tensor_copy(idxf[:], idxs[:]), r=[idxs], w=[idxf]); yield
        i4 = idxf[:, :, :].rearrange("p (h a) k -> p h a k", a=2)
        iot = iota16[:, :].unsqueeze(1).unsqueeze(1).to_broadcast([128, 8, 16, 16])
        for a in range(2):
            sel = ijf[:, a, :].rearrange("p (h k) -> p h k", h=8)
            op('dve', lambda e: e.tensor_tensor(eqv, _bc(sel, 3, [128, 8, 16, 16]), iot, ALU.is_equal), r=[ijf, iota16], w=[eq]); yield
            op('dve', lambda e: e.tensor_tensor(eqv, eqv, _bc(i4[:, :, a, :], 2, [128, 8, 16, 16]), ALU.mult), r=[eq, idxf], w=[eq]); yield
            op('dve', lambda e: e.tensor_reduce(I12[:, a, :].rearrange("p (h k) -> p h k", h=8), eqv, AX.X, ALU.add), r=[eq], w=[I12]); yield
        op('dve', lambda e: e.scalar_tensor_tensor(eidf[:], I12[:, 0, :], 128.0, I12[:, 1, :], ALU.mult, ALU.add), r=[I12], w=[eidf]); yield
        op('dve', lambda e: e.tensor_copy(eidx[:], eidf[:]), r=[eidf], w=[eidx]); yield
        op('dve', lambda e: e.tensor_tensor(gw[:], tv[:], _bc(tv[:, :, 0], 2, [128, 8, 16]), ALU.subtract), r=[tv], w=[gw]); yield
        op('act', lambda e: e.activation(gw[:], gw[:], AF.Exp), r=[gw], w=[gw])
        op('dve', lambda e: e.tensor_reduce(sm[:, 0:8], gw[:], AX.X, ALU.add), r=[gw], w=[sm]); yield
        op('dve', lambda e: e.reciprocal(sm[:, 8:16], sm[:, 0:8]), r=[sm], w=[sm]); yield
        op('dve', lambda e: e.tensor_tensor(gw[:], gw[:], _bc(sm[:, 8:16], 2, [128, 8, 16]), ALU.mult), r=[gw, sm], w=[gw]); yield

    def drain(g):
        if g is not None:
            for _ in g:
                pass

    def step(g, n=1):
        if g is not None:
            for _ in range(n):
                try:
                    next(g)
                except StopIteration:
                    return

    if nown > 0:
        load(0)
        drain(front(0))
    for j in range(nown):
        nxt = None
        if j + 1 < nown:
            load(j + 1)
            nxt = front(j + 1)
            if not pipeline:
                drain(nxt); nxt = None
        x1 = x1b[j % 2]; h2b = h2bs[j % 2]; eidx = eidxs[j % 2]; gw = gws[j % 2]
        gwf = gw[:, :, :].rearrange("p h k -> p (h k)")

        def gather(s, jb=j):
            b = gb[s % NB]
            ei = eidxs[jb % 2]
            op('pool', lambda e: e.indirect_dma_start(out=b[:], out_offset=None, in_=uvb,
                                                      in_offset=bass.IndirectOffsetOnAxis(ap=ei[:, s:s + 1], axis=0)),
               r=[ei], w=[b], dma=True)
        def finish(g0):
            gs_ = slice(g0, g0 + GS)
            op('dve', lambda e: e.tensor_tensor(wgt[:, gs_], ga[:, gs_], gwf[:, gs_], ALU.mult), r=[ga, gw], w=[wgt])
            for s in range(g0, g0 + GS):
                b = gb[s % NB]; d = dg[s % 4]
                if True:
                    op('act', lambda e: e.activation(d[:], identb[:], AF.Identity, scale=wgt[:, s:s + 1]), r=[identb, wgt], w=[d])
                else:
                    op('dve', lambda e: e.tensor_scalar(d[:], identb[:], wgt[:, s:s + 1], None, ALU.mult), r=[identb, wgt], w=[d])
                for hf in range(2):
                    op('pe', lambda e: e.matmul(pacc[:, hf * 512:(hf + 1) * 512], d[:], b[:, 1024 + hf * 512:1024 + (hf + 1) * 512],
                                                start=(s == 0), stop=(s == 127)), r=[d, b], w=["E3"])
        if j == 0:
            for s in range(LEAD):
                gather(s)
        prev = None
        for g0 in range(0, 128, GS):
            for s in range(g0, g0 + GS):
                if s + LEAD < 128:
                    gather(s + LEAD)
                b = gb[s % NB]
                pr_ = prod[s % 4]
                if s % GS == GS - 1:
                    op('dve', lambda e: e.scalar_tensor_tensor(pr_[:], b[:, 0:1024], 1.0, h2b[:], ALU.mult, ALU.mult,
                                                               accum_out=a_t[:, s:s + 1]), r=[b, h2b], w=[pr_, "a_t_dve"])
                else:
                    op('dve', lambda e: e.tensor_tensor(pr_[:], b[:, 0:1024], h2b[:], ALU.mult), r=[b, h2b], w=[pr_])
                    op('act', lambda e: e.activation(pr_[:], pr_[:], AF.Identity, accum_out=a_t[:, s:s + 1]),
                       r=[pr_], w=([pr_, "a_t_act"] if s % GS == GS - 2 else [pr_]))
                step(nxt, 1 + (s % 2))
            gs_ = slice(g0, g0 + GS)
            op('act', lambda e: e.activation(ga[:, gs_], a_t[:, gs_], AF.Gelu), r=["a_t_act", "a_t_dve"], w=[ga])
            if prev is not None:
                finish(prev)
            prev = g0
        finish(prev)
        drain(nxt)
        if j + 1 < nown:
            for s in range(LEAD):
                gather(s, j + 1)
        op('dve', lambda e: e.tensor_tensor(yt[:], pacc[:, :], gate2_bc[:], ALU.mult), r=["E3", gate2_bc], w=[yt])
        op('dve', lambda e: e.tensor_tensor(yt[:], yt[:], x1[:], ALU.add), r=[yt, x1], w=[yt])
        op('act', lambda e: e.activation(xo[:], yt[:], AF.Square, accum_out=stt[:, 4:5]), r=[yt], w=[stt, xo])
        op('dve', lambda e: e.tensor_scalar(stt[:, 5:6], stt[:, 4:5], 1.0 / D_MODEL, EPS, ALU.mult, ALU.add), r=[stt], w=[stt])
        op('act', lambda e: e.activation(stt[:, 7:8], stt[:, 5:6], AF.Ln), r=[stt], w=[stt])
        op('act', lambda e: e.activation(stt[:, 6:7], stt[:, 7:8], AF.Exp, scale=-0.5), r=[stt], w=[stt])
        op('act', lambda e: e.activation(xo[:], yt[:], AF.Identity, scale=stt[:, 6:7]), r=[yt, stt], w=[xo])
        op('dve', lambda e: e.tensor_tensor(yt[:], xo[:], gf_bc[:], ALU.mult), r=[xo, gf_bc], w=[yt])
        c.dma('sp', out[j * 128:(j + 1) * 128, :], yt[:], r=[yt], out_final=True)
    c.barrier()
    S2.close()


_ROPE_CACHE = {}


def _rope_tables(half):
    if half in _ROPE_CACHE:
        return _ROPE_CACHE[half]
    t = np.arange(8192)
    rho = t // 64
    col = t % 64
    row = rho if half == 0 else 127 - rho
    freqs = (np.float32(10000.0) ** (-np.arange(16, dtype=np.float32) / np.float32(16))).astype(np.float32)
    ar = row.astype(np.float32)[:, None] * freqs[None, :]
    ac = col.astype(np.float32)[:, None] * freqs[None, :]
    cr, sr, cc, sc = np.cos(ar), np.sin(ar), np.cos(ac), np.sin(ac)
    cos64 = np.concatenate([cr, cr, cc, cc], axis=1)
    sin64 = np.concatenate([-sr, sr, -sc, sc], axis=1)
    tab = np.stack([cos64, sin64], axis=1).astype(np.float32).reshape(64, 128, 2, 64)
    _ROPE_CACHE[half] = np.ascontiguousarray(tab)
    return _ROPE_CACHE[half]


def _nbias_tables(rpb, half):
    out = np.full((3, 128, 8, 640), NEG, dtype=np.float32)
    p = np.arange(128)
    a, cq = p // 64, p % 64
    kk = np.arange(640)
    kro, ck = kk // 64, kk % 64
    for j in range(3):
        w0 = min(max(2 * j - 4, 0), 58)
        rho = 2 * j + a
        kap = w0 + kro
        if half == 0:
            r, kr = rho, kap
        else:
            r, kr = 127 - rho, 127 - kap
        rs = np.clip(r - 4, 0, 120)
        cs = np.clip(cq - 8, 0, 48)
        vr = (kr[None, :] >= rs[:, None]) & (kr[None, :] <= rs[:, None] + 7)
        vc = (ck[None, :] >= cs[:, None]) & (ck[None, :] <= cs[:, None] + 15)
        valid = vr & vc
        dri = np.clip(kr[None, :] - r[:, None] + 7, 0, 14)
        dci = np.clip(ck[None, :] - cq[:, None] + 15, 0, 30)
        g = rpb[:, dri, dci]
        g = np.transpose(g, (1, 0, 2))
        out[j] = np.where(valid[:, None, :], g, np.float32(NEG))
    return out


_QPERM = np.array([(4 * g + i) * 64 + d for i in range(4) for g in range(2) for d in range(64)])


def _fm(v):
    return np.ascontiguousarray(np.asarray(v, dtype=np.float32).reshape(8, 128).T)


def make_in_maps(inputs):
    f = lambda k: np.asarray(inputs[k], dtype=np.float32)
    x = f("x"); cc = f("c")
    w_in = f("w_in")[0].copy()
    w_in[:, :512] = w_in[:, _QPERM]
    gn = np.concatenate([f("group_norm_a_g")[0], f("group_norm_b_g")[0]])
    shared = {
        "w_ada": np.ascontiguousarray(f("w_ada")[0]), "b_ada": np.ascontiguousarray(f("b_ada")[0][None, :]),
        "g1T": _fm(f("norm1_g")[0]), "g2row": np.ascontiguousarray(f("norm2_g")[0][None, :]),
        "gfrow": np.ascontiguousarray(f("final_norm_g")[None, :]), "w_in": np.ascontiguousarray(w_in),
        "qg": np.ascontiguousarray(f("q_norm_g")[0][None, :]), "kg": np.ascontiguousarray(f("k_norm_g")[0][None, :]),
        "gnT": _fm(gn), "w_out": np.ascontiguousarray(f("w_out")[0]), "w_q": np.ascontiguousarray(f("peer_w_query")[0]),
        "k1T": np.ascontiguousarray(f("peer_sub_keys_1")[0].T), "k2T": np.ascontiguousarray(f("peer_sub_keys_2")[0].T),
        "peer_uv": np.ascontiguousarray(np.stack([f("peer_u")[0], f("peer_v")[0]], axis=1).reshape(16384, 2048)),
    }
    rpb = f("natten_rpb")[0]
    nb = [_nbias_tables(rpb, h) for h in range(2)]
    maps = []
    for core in range(8):
        b, half = core // 2, core % 2
        xb = x[b]
        if half == 1:
            xb = xb.reshape(128, 64, 1024)[::-1].reshape(8192, 1024)
        m = dict(shared)
        m["xs"] = np.ascontiguousarray(xb)
        m["cT"] = _fm(cc[b])
        m["rope"] = _rope_tables(half)
        m["nbias"] = nb[half]
        maps.append(m)
    return maps


def assemble(results):
    out = np.empty((4, 8192, 1024), dtype=np.float32)
    for core in range(8):
        b, half = core // 2, core % 2
        o = np.asarray(results[core]["out"], dtype=np.float32).reshape(64, 64, 1024)
        if half == 0:
            out[b, :4096] = o.reshape(4096, 1024)
        else:
            out[b].reshape(128, 64, 1024)[64:] = o[::-1]
    return out


_NC_CACHE = {}


def kernel(**inputs):
    if "nc" not in _NC_CACHE:
        _NC_CACHE["nc"] = build_nc()
    nc = _NC_CACHE["nc"]
    maps = make_in_maps(inputs)
    res = run_bass_kernel_spmd(nc, maps, core_ids=list(range(8)))
    return assemble(res.results)
```

```python
import numpy as np
import ml_dtypes
import concourse.bass as bass
import concourse.mybir as mybir
from concourse.bass_utils import run_bass_kernel_spmd

F32 = mybir.dt.float32
BF16 = mybir.dt.bfloat16
I32 = mybir.dt.int32
U32 = mybir.dt.uint32
AF = mybir.ActivationFunctionType
ALU = mybir.AluOpType
AX = mybir.AxisListType


class Ctx:
    def __init__(self, nc):
        self.nc = nc
        self.eng = {'pe': nc.tensor, 'dve': nc.vector, 'act': nc.scalar, 'pool': nc.gpsimd, 'sp': nc.sync}
        self.sem = {k: nc.alloc_semaphore("s_" + k) for k in self.eng}
        self.cnt = {k: 0 for k in self.eng}
        self.know = {k: {} for k in self.eng}
        self.clock = {}
        self.last_w = {}
        self.readers = {}
        self.chan = {}
        self.names = {}
        self.n_wait = 0
        self.n_ins = 0
        self.out_events = []

    def sb(self, name, shape, dt):
        t = self.nc.alloc_sbuf_tensor(name, list(shape), dt)
        self.names[id(t)] = name
        return t

    def ps(self, name, shape, dt=F32):
        t = self.nc.alloc_psum_tensor(name, list(shape), dt)
        self.names[id(t)] = name
        return t

    def key(self, k):
        if isinstance(k, str):
            return k
        return self.names[id(k)]

    def _semof(self, src):
        if src in self.sem:
            return self.sem[src]
        return self.chan[src][0]

    def _need(self, e, ev, needs):
        if ev is None:
            return
        src, val = ev
        if src == 'pe' and e == 'pe':
            return
        if self.know[e].get(src, 0) >= val:
            return
        if needs.get(src, 0) < val:
            needs[src] = val

    def _collect(self, e, r, w):
        needs = {}
        for k in r:
            self._need(e, self.last_w.get(self.key(k)), needs)
        for k in w:
            kk = self.key(k)
            self._need(e, self.last_w.get(kk), needs)
            for src, val in self.readers.get(kk, {}).items():
                self._need(e, (src, val), needs)
        items = list(needs.items())
        for src, val in items:
            ck = self.clock.get((src, val), {})
            for s2, v2 in list(needs.items()):
                if s2 != src and ck.get(s2, 0) >= v2:
                    del needs[s2]
        for src, val in needs.items():
            self.eng[e].wait_ge(self._semof(src), val)
            self.n_wait += 1
            kn = self.know[e]
            if kn.get(src, 0) < val:
                kn[src] = val
            for s2, v2 in self.clock.get((src, val), {}).items():
                if kn.get(s2, 0) < v2:
                    kn[s2] = v2

    def _record(self, ev, r, w):
        for k in r:
            kk = self.key(k)
            d = self.readers.setdefault(kk, {})
            if d.get(ev[0], 0) < ev[1]:
                d[ev[0]] = ev[1]
        for k in w:
            kk = self.key(k)
            self.last_w[kk] = ev
            self.readers[kk] = {}

    cut = None

    def op(self, e, fn, r=(), w=(), dma=False, ch=None):
        if self.cut is not None and self.n_ins >= self.cut:
            return None
        if dma:
            return self._dma(e, fn, r, w, ch)
        self._collect(e, r, w)
        ins = fn(self.eng[e])
        self.cnt[e] += 1
        ins.then_inc(self.sem[e], 1)
        ev = (e, self.cnt[e])
        ck = dict(self.know[e])
        self.clock[ev] = ck
        self._record(ev, r, w)
        self.n_ins += 1
        return ev

    def _dma(self, q, fn, r, w, ch=None):
        self._collect(q, r, w)
        if ch is None:
            ch = self.key(w[0]) if len(w) else self.key(r[0])
        ch = "dma_" + ch + ("_sw" if q == 'pool' else "")
        if ch not in self.chan:
            self.chan[ch] = [self.nc.alloc_semaphore(ch), 0]
        ins = fn(self.eng[q])
        self.chan[ch][1] += 16
        ins.then_inc(self.chan[ch][0], 16)
        ev = (ch, self.chan[ch][1])
        self.clock[ev] = dict(self.know[q])
        self._record(ev, r, w)
        self.n_ins += 1
        return ev

    def dma(self, q, out, in_, r=(), w=(), ch=None, out_final=False):
        ev = self._dma(q, lambda e: e.dma_start(out=out, in_=in_), r, w, ch)
        if out_final:
            self.out_events.append(ev)
        return ev

    def finish(self):
        needs = {}
        for ev in self.out_events:
            if needs.get(ev[0], 0) < ev[1]:
                needs[ev[0]] = ev[1]
        for ch, (sem, count) in self.chan.items():
            if count > 0 and needs.get(ch, 0) < count:
                needs[ch] = count
        for src, val in needs.items():
            self.eng['sp'].wait_ge(self._semof(src), val)
        for e in ('pe', 'dve', 'act', 'pool'):
            if self.cnt[e] > 0:
                self.eng['sp'].wait_ge(self.sem[e], self.cnt[e])


def make_ident(c, identf, identb=None):
    n = 128
    nc = c.nc
    col = c.sb("mk_col", [n, n], F32)
    row = c.sb("mk_row", [n, 1], F32)
    c.op('pool', lambda e: e.iota(col[:], pattern=[[1, n]], base=0, channel_multiplier=0,
                                  allow_small_or_imprecise_dtypes=True), w=[col])
    c.op('pool', lambda e: e.iota(row[:], pattern=[[0, 1]], base=0, channel_multiplier=1,
                                  allow_small_or_imprecise_dtypes=True), w=[row])
    c.op('dve', lambda e: e.tensor_scalar(identf[:], col[:], row[:, 0:1], None, ALU.is_equal), r=[col, row], w=[identf])
    if identb is not None:
        c.op('dve', lambda e: e.tensor_copy(identb[:], identf[:]), r=[identf], w=[identb])


def _barrier(c):
    for e in ('pe', 'dve', 'act', 'pool', 'sp'):
        kn = c.know[e]
        for src in ('pe', 'dve', 'act', 'pool'):
            if c.cnt[src] > 0 and kn.get(src, 0) < c.cnt[src]:
                c.eng[e].wait_ge(c.sem[src], c.cnt[src])
                kn[src] = c.cnt[src]
        for ch, (sem, count) in c.chan.items():
            if count > 0 and kn.get(ch, 0) < count:
                c.eng[e].wait_ge(sem, count)
                kn[ch] = count


Ctx.barrier = _barrier

D_MODEL = 1024
NBLK_ALL = 64
NBLK_KVB = 34
EPS = 1e-6
NEG = -30000.0


class _Scope:
    def __init__(self, c, prefix):
        import contextlib
        self.c = c
        self.prefix = prefix
        self.stack = contextlib.ExitStack()

    def sb(self, name, shape, dt):
        t = self.stack.enter_context(self.c.nc.sbuf_tensor(self.prefix + name, list(shape), dt))
        self.c.names[id(t)] = self.prefix + name
        return t

    def ps(self, name, shape, dt=F32):
        t = self.stack.enter_context(self.c.nc.psum_tensor(self.prefix + name, list(shape), dt))
        self.c.names[id(t)] = self.prefix + name
        return t

    def close(self):
        self.stack.close()


def _bc(ap, axis, shape):
    return ap.unsqueeze(axis).to_broadcast(list(shape))


def build_nc(nown=32, nkv=NBLK_ALL, stop_after=None, dbg=0):
    nc = bass.Bass("TRN2", target_bir_lowering=False)
    DI = lambda name, shape, dt=F32: nc.dram_tensor(name, list(shape), dt, kind="ExternalInput").ap()
    xs = DI("xs", [8192, 1024])
    cT_d = DI("cT", [128, 8])
    w_ada = DI("w_ada", [1024, 6144])
    b_ada = DI("b_ada", [1, 6144])
    g1T_d = DI("g1T", [128, 8])
    g2row = DI("g2row", [1, 1024])
    gfrow = DI("gfrow", [1, 1024])
    w_in = DI("w_in", [1024, 2304])
    qg_d = DI("qg", [1, 64])
    kg_d = DI("kg", [1, 64])
    gnT_d = DI("gnT", [128, 8])
    w_out = DI("w_out", [1024, 1024])
    w_q = DI("w_q", [1024, 2048])
    k1T_d = DI("k1T", [128, 128])
    k2T_d = DI("k2T", [128, 128])
    peer_uv = DI("peer_uv", [16384, 2048])
    uvb = nc.dram_tensor("uvb_scr", [16384, 2048], BF16).ap()
    rope_d = DI("rope", [NBLK_ALL, 128, 2, 64])
    nbias_d = DI("nbias", [3, 128, 8, 640])
    out = nc.dram_tensor("out", [4096, 1024], F32, kind="ExternalOutput").ap()
    kbt_dram = nc.dram_tensor("kbt_scr", [NBLK_KVB, 128, 4, 128], BF16).ap()
    vb_dram = nc.dram_tensor("vb_scr", [NBLK_KVB, 128, 512], BF16).ap()

    c = Ctx(nc)
    op = c.op

    identf = c.sb("identf", [128, 128], F32)
    identb = c.sb("identb", [128, 128], BF16)
    make_ident(c, identf, identb)
    neghalf = c.sb("neghalf", [128, 8], F32)
    op('dve', lambda e: e.memset(neghalf[:], -0.5), w=[neghalf])
    iota16 = c.sb("iota16", [128, 16], F32)
    op('pool', lambda e: e.iota(iota16[:], pattern=[[1, 16]], base=0, channel_multiplier=0,
                                allow_small_or_imprecise_dtypes=True), w=[iota16])
    gate1_bc = c.sb("gate1_bc", [128, 1024], F32)
    gate2_bc = c.sb("gate2_bc", [128, 1024], F32)
    A2_bc = c.sb("A2_bc", [128, 1024], F32)
    B2_bc = c.sb("B2_bc", [128, 1024], F32)
    gf_bc = c.sb("gf_bc", [128, 1024], F32)
    modT = c.sb("modT", [128, 2, 8], F32)
    A1 = c.sb("A1", [128, 8], F32)
    g1T = c.sb("g1T_t", [128, 8], F32)
    gnT = c.sb("gnT_t", [128, 8], F32)
    qg = c.sb("qg_t", [128, 64], F32)
    kg = c.sb("kg_t", [128, 64], F32)
    negC = c.sb("negC", [128, 1], F32)
    k1T = c.sb("k1T_t", [128, 128], BF16)
    k2T = c.sb("k2T_t", [128, 128], BF16)

    c.dma('sp', g1T[:], g1T_d, w=[g1T])
    c.dma('sp', gnT[:], gnT_d, w=[gnT])
    c.dma('sp', qg[:], qg_d.to_broadcast([128, 64]), w=[qg])
    c.dma('sp', kg[:], kg_d.to_broadcast([128, 64]), w=[kg])
    c.dma('sp', gf_bc[:], gfrow.to_broadcast([128, 1024]), w=[gf_bc])
    c.dma('sp', A2_bc[:], g2row.to_broadcast([128, 1024]), w=[A2_bc])
    op('pool', lambda e: e.dma_start(out=k1T[:], in_=k1T_d), w=[k1T], dma=True)
    op('pool', lambda e: e.dma_start(out=k2T[:], in_=k2T_d), w=[k2T], dma=True)

    mq = c.sb("mq", [128, 2], F32)
    op('dve', lambda e: e.tensor_reduce(mq[:, 0:1], qg[:], AX.X, ALU.max, apply_absolute_value=True), r=[qg], w=[mq])
    op('dve', lambda e: e.tensor_reduce(mq[:, 1:2], kg[:], AX.X, ALU.max, apply_absolute_value=True), r=[kg, mq], w=[mq])
    op('dve', lambda e: e.scalar_tensor_tensor(negC[:], mq[:, 0:1], -8.0, mq[:, 1:2], ALU.mult, ALU.mult), r=[mq], w=[negC])

    S0 = _Scope(c, "s0_")
    cT = S0.sb("cT", [128, 8], F32)
    sc = S0.sb("sc", [128, 8], F32)
    scbc = S0.sb("scbc", [128, 8, 128], F32)
    wada = [S0.sb("wada%d" % i, [128, 8, 512], F32) for i in range(2)]
    bb = [S0.sb("bb%d" % i, [128, 512], F32) for i in range(2)]
    modg = [S0.sb("modg%d" % i, [128, 512], F32) for i in range(2)]
    psm = [S0.ps("psm%d" % i, [128, 512], F32) for i in range(2)]
    pst = S0.ps("pst", [128, 4, 128], F32)
    c.dma('sp', cT[:], cT_d, w=[cT])
    op('act', lambda e: e.activation(sc[:], cT[:], AF.Silu), r=[cT], w=[sc])
    op('dve', lambda e: e.tensor_copy(scbc[:], _bc(sc[:], 2, [128, 8, 128])), r=[sc], w=[scbc])
    w_ada_v = w_ada.rearrange("(j p) n -> p j n", p=128)
    for gi in range(12):
        wt = wada[gi % 2]; bt = bb[gi % 2]; mg = modg[gi % 2]; pm = psm[gi % 2]
        c.dma('sp', wt[:], w_ada_v[:, :, gi * 512:(gi + 1) * 512], w=[wt])
        c.dma('sp', bt[:], b_ada[0:1, gi * 512:(gi + 1) * 512].to_broadcast([128, 512]), w=[bt])
        for j in range(8):
            op('pe', lambda e: e.matmul(pm[:], scbc[:, j, :], wt[:, j, :], start=(j == 0), stop=(j == 7)),
               r=[scbc, wt], w=[pm])
        piece, half = gi // 2, gi % 2
        hs = slice(half * 512, (half + 1) * 512)
        if piece in (0, 1):
            op('dve', lambda e: e.tensor_tensor(mg[:], pm[:], bt[:], ALU.add), r=[pm, bt], w=[mg])
            for k in range(4):
                op('pe', lambda e: e.transpose(pst[:, k, :], mg[:, k * 128:(k + 1) * 128], identf[:]),
                   r=[mg, identf], w=[pst])
            op('dve', lambda e: e.tensor_copy(modT[:, piece, half * 4:(half + 1) * 4], pst[:, :, 0]), r=[pst], w=[modT])
        else:
            dst = {2: gate1_bc, 3: B2_bc, 4: None, 5: gate2_bc}[piece]
            if dst is not None:
                op('dve', lambda e: e.tensor_tensor(dst[:, hs], pm[:], bt[:], ALU.add), r=[pm, bt], w=[dst])
            else:
                op('dve', lambda e: e.tensor_tensor(mg[:], pm[:], bt[:], ALU.add), r=[pm, bt], w=[mg])
                op('dve', lambda e: e.scalar_tensor_tensor(A2_bc[:, hs], mg[:], 1.0, A2_bc[:, hs], ALU.add, ALU.mult),
                   r=[mg, A2_bc], w=[A2_bc])
    op('dve', lambda e: e.scalar_tensor_tensor(A1[:], modT[:, 1, :], 1.0, g1T[:], ALU.add, ALU.mult), r=[modT, g1T], w=[A1])
    c.barrier()
    S0.close()
    if stop_after == 'S0':
        c.dma('sp', out[0:128, :], gate1_bc[:], r=[gate1_bc], out_final=True)
        c.dma('sp', out[128:256, :], A2_bc[:], r=[A2_bc], out_final=True)
        c.dma('sp', out[256:384, 0:8], A1[:], r=[A1], out_final=True)
        c.dma('sp', out[256:384, 8:24], modT[:, :, :].rearrange("p a b -> p (a b)"), r=[modT], out_final=True)
        c.finish()
        return nc

    S1 = _Scope(c, "s1_")
    Win = S1.sb("Win", [128, 8, 2304], BF16)
    Wout = S1.sb("Wout", [128, 8, 1024], BF16)
    KAT = S1.sb("KAT", [128, 8192], BF16)
    VA = S1.sb("VA", [128, NBLK_ALL, 2, 65], BF16)
    xbuf = [S1.sb("x%d" % i, [128, 1024], F32) for i in range(2)]
    ropeb = [S1.sb("rope%d" % i, [128, 2, 64], F32) for i in range(2)]
    xn = S1.sb("xn", [128, 1024], BF16)
    hT = S1.sb("hT", [128, 8, 128], BF16)
    st1 = S1.sb("st1", [128, 16], F32)
    D = [S1.ps("D%d" % i, [128, 1024], F32) for i in range(4)]
    Dk = lambda i, h: "D%d%s" % (i, "ab"[h])

    w_in_v = w_in.rearrange("(j p) n -> p j n", p=128)
    for k in range(3):
        op('pool', lambda e: e.dma_start(out=Win[:, :, k * 768:(k + 1) * 768], in_=w_in_v[:, :, k * 768:(k + 1) * 768]),
           w=[Win], dma=True)
    op('pool', lambda e: e.dma_start(out=Wout[:], in_=w_out.rearrange("(j p) n -> p j n", p=128)), w=[Wout], dma=True)
    op('dve', lambda e: e.memset(VA[:, :, :, 64:65], 1.0), w=[VA])

    A1b = _bc(A1[:], 2, [128, 8, 128])
    B1b = _bc(modT[:, 0, :], 2, [128, 8, 128])
    pTv = D[0][:, 0:512].bitcast(BF16).rearrange("p (j n) -> p j n", j=8)

    def load_x(i):
        c.dma('sp', xbuf[i % 2][:], xs[i * 128:(i + 1) * 128, :], w=[xbuf[i % 2]])
        c.dma('sp', ropeb[i % 2][:], rope_d[i], w=[ropeb[i % 2]])

    def norm_hT_g(xt):
        op('act', lambda e: e.activation(xn[:], xt[:], AF.Square, accum_out=st1[:, 0:1]), r=[xt], w=[st1, xn]); yield
        op('dve', lambda e: e.tensor_scalar(st1[:, 1:2], st1[:, 0:1], 1.0 / D_MODEL, EPS, ALU.mult, ALU.add), r=[st1], w=[st1]); yield
        op('act', lambda e: e.activation(st1[:, 3:4], st1[:, 1:2], AF.Ln), r=[st1], w=[st1]); yield
        op('act', lambda e: e.activation(st1[:, 2:3], st1[:, 3:4], AF.Exp, scale=-0.5), r=[st1], w=[st1]); yield
        op('act', lambda e: e.activation(xn[:], xt[:], AF.Identity, scale=st1[:, 2:3]), r=[xt, st1], w=[xn]); yield
        for j in range(8):
            op('pe', lambda e: e.transpose(pTv[:, j, :], xn[:, j * 128:(j + 1) * 128], identb[:]), r=[xn, identb], w=["D0a"])
        yield
        op('dve', lambda e: e.tensor_tensor(hT[:], pTv, A1b, ALU.mult), r=["D0a", A1], w=[hT]); yield
        op('dve', lambda e: e.tensor_tensor(hT[:], hT[:], B1b, ALU.add), r=[hT, modT], w=[hT]); yield

    def norm_hT(xt):
        for _ in norm_hT_g(xt):
            pass

    def head_norm_rope_g(dst_bf, src_sb, H, gain, rp, scr):
        sq, ssq, tmp, t1, t2 = scr
        sv = src_sb[:, 0:H * 64].rearrange("p (h d) -> p h d", h=H)
        sqv = sq[:, 0:H * 64].rearrange("p (h d) -> p h d", h=H)
        op('dve', lambda e: e.tensor_tensor(sqv, sv, sv, ALU.mult), r=[src_sb], w=[sq])
        yield
        op('dve', lambda e: e.tensor_reduce(ssq[:, 0:H], sqv, AX.X, ALU.add), r=[sq], w=[ssq])
        yield
        op('dve', lambda e: e.tensor_scalar(ssq[:, 8:8 + H], ssq[:, 0:H], 1.0 / 64, EPS, ALU.mult, ALU.add), r=[ssq], w=[ssq])
        yield
        op('act', lambda e: e.activation(ssq[:, 0:H], ssq[:, 8:8 + H], AF.Ln), r=[ssq], w=[ssq])
        yield
        op('act', lambda e: e.activation(ssq[:, 16:16 + H], ssq[:, 0:H], AF.Exp, scale=-0.5), r=[ssq], w=[ssq])
        yield
        tv = tmp[:, 0:H * 64].rearrange("p (h d) -> p h d", h=H)
        op('dve', lambda e: e.tensor_tensor(tv, sv, _bc(ssq[:, 16:16 + H], 2, [128, H, 64]), ALU.mult), r=[src_sb, ssq], w=[tmp])
        yield
        op('dve', lambda e: e.tensor_tensor(tv, tv, _bc(gain[:], 1, [128, H, 64]), ALU.mult), r=[tmp, gain], w=[tmp])
        yield
        t1v = t1[:, 0:H * 64].rearrange("p (h d) -> p h d", h=H)
        op('dve', lambda e: e.tensor_tensor(t1v, tv, _bc(rp[:, 0, :], 1, [128, H, 64]), ALU.mult), r=[tmp, rp], w=[t1])
        yield
        x5 = tmp[:, 0:H * 64].rearrange("p (h a b d) -> p h a b d", h=H, a=2, b=2)
        o5 = t2[:, 0:H * 64].rearrange("p (h a b d) -> p h a b d", h=H, a=2, b=2)
        s5 = rp[:, 1, :].rearrange("p (a b d) -> p a b d", a=2, b=2)
        for b_ in range(2):
            op('dve', lambda e: e.tensor_tensor(o5[:, :, :, b_, :], x5[:, :, :, 1 - b_, :],
                                                _bc(s5[:, :, b_, :], 1, [128, H, 2, 16]), ALU.mult), r=[tmp, rp], w=[t2])
            yield
        op('dve', lambda e: e.tensor_tensor(dst_bf[:, 0:H * 64], t1[:, 0:H * 64], t2[:, 0:H * 64], ALU.add), r=[t1, t2], w=[dst_bf])
        yield


    def head_norm_rope(dst_bf, src_sb, H, gain, rp, scr):
        for _ in head_norm_rope_g(dst_bf, src_sb, H, gain, rp, scr):
            pass

    ka_sb = S1.sb("ka_sb", [128, 128], F32)
    scrA = (S1.sb("sqA", [128, 512], F32), S1.sb("ssqA", [128, 24], F32), S1.sb("tmpA", [128, 512], F32),
            S1.sb("t1A", [128, 512], F32), S1.sb("t2A", [128, 512], F32))
    kr = S1.sb("kr", [128, 128], BF16)
    kbs = [S1.sb("kbs%d" % i, [128, 4, 128], BF16) for i in range(2)]
    vbs = [S1.sb("vbs%d" % i, [128, 512], BF16) for i in range(2)]
    pA = D[0][:, 512:768]
    pKv = D[1][:, 0:64].bitcast(BF16)
    pKB = D[2][:, 0:512].rearrange("p (f n) -> p f n", f=4)
    pVB = D[3][:, 0:512]
    cvb = [S1.sb("cv%d" % i, [128, 2048], BF16) for i in range(2)]
    n_cv = 128
    per_blk = -(-n_cv // nkv)

    def convert(k):
        cv = cvb[k % 2]
        op('pool', lambda e: e.dma_start(out=cv[:], in_=peer_uv[k * 128:(k + 1) * 128, :]), w=[cv], dma=True)
        c.dma('sp', uvb[k * 128:(k + 1) * 128, :], cv[:], r=[cv])
    load_x(0)
    for i in range(nkv):
        if i + 1 < nkv:
            load_x(i + 1)
        for k in range(i * per_blk, min((i + 1) * per_blk, n_cv)):
            convert(k)
        xt = xbuf[i % 2]; rp = ropeb[i % 2]
        norm_hT(xt)
        for j in range(8):
            op('pe', lambda e: e.matmul(pA, hT[:, j, :], Win[:, j, 512:768], start=(j == 0), stop=(j == 7)), r=[hT, Win], w=["D0b"])
        op('act', lambda e: e.copy(ka_sb[:], pA[:, 0:128]), r=["D0b"], w=[ka_sb])
        op('act', lambda e: e.copy(VA[:, i, :, 0:64], pA[:, 128:256].rearrange("p (g d) -> p g d", g=2)), r=["D0b"], w=[VA])
        head_norm_rope(kr, ka_sb, 2, kg, rp, scrA)
        op('pe', lambda e: e.transpose(pKv, kr[:], identb[:]), r=[kr, identb], w=["D1a"])
        op('act', lambda e: e.copy(KAT[:, i * 128:(i + 1) * 128], pKv), r=["D1a"], w=[KAT])
        if i < NBLK_KVB:
            for f in range(4):
                for j in range(8):
                    op('pe', lambda e: e.matmul(pKB[:, f, :], Win[:, j, 1280 + f * 128:1280 + (f + 1) * 128], hT[:, j, :],
                                                start=(j == 0), stop=(j == 7)), r=[hT, Win], w=["D2a"])
            ks = kbs[i % 2]; vs = vbs[i % 2]
            op('act', lambda e: e.copy(ks[:], pKB), r=["D2a"], w=[ks])
            c.dma('sp', kbt_dram[i], ks[:], r=[ks])
            for j in range(8):
                op('pe', lambda e: e.matmul(pVB, hT[:, j, :], Win[:, j, 1792:2304], start=(j == 0), stop=(j == 7)), r=[hT, Win], w=["D3a"])
            op('dve', lambda e: e.tensor_copy(vs[:], pVB), r=["D3a"], w=[vs])
            c.dma('sp', vb_dram[i], vs[:], r=[vs])
    c.barrier()
    if stop_after == 'A':
        dbg = S1.sb("dbg", [128, 1024], F32)
        c.op('dve', lambda e: e.tensor_copy(dbg[:, 0:256], KAT[:, 0:256]), r=[KAT], w=[dbg])
        c.op('dve', lambda e: e.tensor_copy(dbg[:, 256:516], VA[:, 0:2, :, :].rearrange("p a g d -> p (a g d)")), r=[VA], w=[dbg])
        c.dma('sp', out[0:128, :], dbg[:], r=[dbg], out_final=True)
        dbg2 = S1.sb("dbg2", [128, 1024], BF16)
        c.dma('sp', dbg2[:, 0:512], kbt_dram[1].rearrange("p f t -> p (f t)"), w=[dbg2])
        c.dma('sp', dbg2[:, 512:1024], vb_dram[1], w=[dbg2])
        dbg3 = S1.sb("dbg3", [128, 1024], F32)
        c.op('dve', lambda e: e.tensor_copy(dbg3[:], dbg2[:]), r=[dbg2], w=[dbg3])
        c.dma('sp', out[128:256, :], dbg3[:], r=[dbg3], out_final=True)
        c.finish()
        return nc

    kwin = [S1.sb("kwin%d" % i, [128, 5, 4, 128], BF16) for i in range(1)]
    vwin = [S1.sb("vwin%d" % i, [128, 5, 512], BF16) for i in range(1)]
    nbt = S1.sb("nbt", [128, 8, 640], F32)
    qa_sb = S1.sb("qa_sb", [128, 512], F32)
    qr = S1.sb("qr", [128, 512], BF16)
    qb = S1.sb("qb", [128, 512], BF16)
    qAT = [S1.sb("qAT%d" % i, [128, 4, 128], BF16) for i in range(2)]
    qBTs = [S1.sb("qBT%d" % i, [128, 4, 128], BF16) for i in range(2)]
    PT2 = [S1.sb("PT2_%d" % i, [128, 1024], BF16) for i in range(3)]
    oT = S1.sb("oT", [65, 2, 512], F32)
    st2 = S1.sb("st2", [128, 48], F32)
    mixf = S1.sb("mixf", [128, 1024], F32)
    mixn = S1.sb("mixn", [128, 1024], BF16)
    mixT = S1.sb("mixT", [128, 8, 128], BF16)
    s_sb = S1.sb("s_sb", [128, 640], F32)
    p_sb = S1.sb("p_sb", [128, 640], BF16)
    PTn = S1.sb("PTn", [128, 5, 128], BF16)
    x1t = [S1.sb("x1t%d" % i, [128, 1024], F32) for i in range(1)]

    def win_start(j):
        return min(max(2 * j - 4, 0), 58) // 2

    def load_win(j):
        cb = win_start(j)
        c.dma('sp', kwin[0][:], kbt_dram[cb:cb + 5].rearrange("b p f t -> p b f t"), w=[kwin[0]])
        c.dma('sp', vwin[0][:], vb_dram[cb:cb + 5].rearrange("b p n -> p b n"), w=[vwin[0]])

    Dp = [D[1], D[2], D[3]]
    Dpk = [("D1a", "D1b"), ("D2a", "D2b"), ("D3a", "D3b")]
    pO = [D[0][0:65, 0:512], D[0][0:65, 512:1024]]
    pOk = ["D0a", "D0b"]
    pT2 = D[0][:, 512:1024].bitcast(BF16).rearrange("p (j n) -> p j n", j=8)
    pTa = D[1][:, :].rearrange("p (s n) -> p s n", s=8)
    import os
    print("B1 start n_ins", c.n_ins)
    if os.environ.get("CUT"):
        c.cut = c.n_ins + int(os.environ["CUT"])
    for g_ in range(2):
        op('dve', lambda e: e.memset(qAT[g_][:], 0.0), w=[qAT[g_]])
    def pre_gen(jb):
        xt_ = xbuf[jb % 2]; rp_ = ropeb[jb % 2]; qBT_ = qBTs[jb % 2]
        yield from norm_hT_g(xt_)
        for hh, c0 in ((0, 0), (1, 768)):
            for jj in range(8):
                op('pe', lambda e: e.matmul(D[3][:, hh * 512:(hh + 1) * 512], hT[:, jj, :], Win[:, jj, c0:c0 + 512],
                                            start=(jj == 0), stop=(jj == 7)), r=[hT, Win], w=[Dk(3, hh)])
            yield
        op('act', lambda e: e.copy(qa_sb[:], D[3][:, 0:512]), r=["D3a"], w=[qa_sb]); yield
        op('act', lambda e: e.copy(qb[:], D[3][:, 512:1024]), r=["D3b"], w=[qb]); yield
        yield from head_norm_rope_g(qr, qa_sb, 8, qg, rp_, scrA)
        for k in range(4):
            op('pe', lambda e: e.transpose(pT2[:, k, :], qr[:, k * 128:(k + 1) * 128], identb[:]), r=[qr, identb], w=["D0b"])
        for k in range(4):
            op('pe', lambda e: e.transpose(pT2[:, 4 + k, :], qb[:, k * 128:(k + 1) * 128], identb[:]), r=[qb, identb], w=["D0b"])
        yield
        for g_ in range(2):
            op('dve', lambda e: e.tensor_copy(qAT[g_][64 * g_:64 * g_ + 64, :, :], pT2[64 * g_:64 * g_ + 64, 0:4, :]), r=["D0b"], w=[qAT[g_]])
            yield
        op('dve', lambda e: e.tensor_copy(qBT_[:], pT2[:, 4:8, :]), r=["D0b"], w=[qBT_]); yield

    def _drain(g):
        if g is not None:
            for _ in g:
                pass

    def _step(g, n):
        if g is not None:
            for _ in range(n):
                try:
                    next(g)
                except StopIteration:
                    return

    if nown > 0:
        load_x(0)
        if not os.environ.get("NO_WIN"):
            load_win(0)
        _drain(pre_gen(0))
    for j in range(nown):
        xt = xbuf[j % 2]; rp = ropeb[j % 2]; qBT = qBTs[j % 2]
        if j + 1 < nown:
            load_x(j + 1)
        if j <= 2 and not os.environ.get("NO_NBT"):
            c.dma('sp', nbt[:], nbias_d[j], w=[nbt])
        for g in range(2):
            pb = 64 * g
            npair = nkv // 2

            def S2(pi):
                for u in range(2):
                    kc = 2 * pi + u
                    op('pe', lambda e: e.matmul(Dp[pi % 3][:, u * 512:(u + 1) * 512], KAT[:, kc * 128:(kc + 1) * 128],
                                                qAT[g][:, :, :], start=True, stop=True), r=[KAT, qAT[g]], w=[Dpk[pi % 3][u]])

            def EXP2(pi):
                op('act', lambda e: e.activation(PT2[pi % 3][:], Dp[pi % 3][:, :], AF.Exp, scale=0.125, bias=negC[:, 0:1]),
                   r=[Dpk[pi % 3][0], Dpk[pi % 3][1], negC], w=[PT2[pi % 3]])

            def PV2(pi):
                for u in range(2):
                    kc = 2 * pi + u
                    op('pe', lambda e: e.matmul(pO[g], VA[:, kc, g, :], PT2[pi % 3][:, u * 512:(u + 1) * 512],
                                                start=(kc == 0), stop=(kc == nkv - 1)), r=[VA, PT2[pi % 3]], w=[pOk[g]])
            S2(0)
            if npair > 1:
                S2(1)
            for pi in range(npair):
                EXP2(pi)
                if pi + 2 < npair:
                    S2(pi + 2)
                PV2(pi)
            op('dve', lambda e: e.tensor_copy(oT[:, g, :], pO[g]), r=[pOk[g]], w=[oT])
        for g in range(2):
            for i in range(4):
                op('pe', lambda e: e.transpose(pTa[:, g * 4 + i, 0:65], oT[0:65, g, i * 128:(i + 1) * 128], identf[0:65, 0:65]),
                   r=[oT, identf], w=["D1a", "D1b"])
        op('dve', lambda e: e.reciprocal(st2[:, 0:8], pTa[:, :, 64]), r=["D1a", "D1b"], w=[st2])
        op('dve', lambda e: e.tensor_tensor(mixf[:, 0:512].rearrange("p (s d) -> p s d", s=8), pTa[:, :, 0:64],
                                            _bc(st2[:, 0:8], 2, [128, 8, 64]), ALU.mult), r=["D1a", "D1b", st2], w=[mixf])
        if dbg == 2:
            c.dma('sp', out[0:128, :], mixf[:], r=[mixf], out_final=True)
            c.finish()
            return nc
        kw = kwin[0]; vw = vwin[0]
        pNs = [D[2], D[2]]
        pNk = [("D2a", "D2b"), ("D2a", "D2b")]
        nxt_pre = pre_gen(j + 1) if j + 1 < nown else None
        pPT = D[1][:, 0:320].bitcast(BF16).rearrange("p (c n) -> p c n", c=5)
        pOB = D[1][:, 512:1024]
        s_sbs = [s_sb[:], cvb[0][:, :].bitcast(F32)[:, 0:640]]
        s_sbk = [s_sb, cvb[0]]
        p_sbs = [p_sb[:], cvb[1][:, 0:640]]
        p_sbk = [p_sb, cvb[1]]

        def na_front(h):
            pr, hb = h // 2, 64 * (h % 2)
            pN = pNs[h % 2]; ss_ = s_sbs[h % 2]; ps_ = p_sbs[h % 2]
            op('pe', lambda e: e.matmul(pN[:, 0:512], qBT[hb:hb + 64, pr, :], kw[hb:hb + 64, 0:4, pr, :], start=True, stop=True),
               r=[qBT, kw], w=[pNk[h % 2][0]])
            op('pe', lambda e: e.matmul(pN[:, 512:640], qBT[hb:hb + 64, pr, :], kw[hb:hb + 64, 4, pr, :], start=True, stop=True),
               r=[qBT, kw], w=[pNk[h % 2][1]])
            op('dve', lambda e: e.scalar_tensor_tensor(ss_, pN[:, 0:640], 0.125, nbt[:, h, :], ALU.mult, ALU.add),
               r=[pNk[h % 2][0], pNk[h % 2][1], nbt], w=[s_sbk[h % 2]])
            op('dve', lambda e: e.tensor_reduce(st2[:, 8 + h % 2:9 + h % 2], ss_, AX.X, ALU.max, negate=True), r=[s_sbk[h % 2]], w=["st2_nm%d" % (h % 2)])
            op('act', lambda e: e.activation(ps_, ss_, AF.Exp, bias=st2[:, 8 + h % 2:9 + h % 2], accum_out=st2[:, 16 + h:17 + h]),
               r=[s_sbk[h % 2], "st2_nm%d" % (h % 2)], w=[p_sbk[h % 2], "st2_rs%d" % h])
        na_front(0)
        for h in range(8):
            if h + 1 < 8:
                na_front(h + 1)
            ps_ = p_sbs[h % 2]
            for cc in range(5):
                op('pe', lambda e: e.transpose(pPT[:, cc, :], ps_[:, cc * 128:(cc + 1) * 128], identb[:]), r=[p_sbk[h % 2], identb], w=["D1a"])
            op('dve', lambda e: e.tensor_copy(PTn[:], pPT), r=["D1a"], w=[PTn])
            for cc in range(5):
                op('pe', lambda e: e.matmul(pOB[:, h * 64:(h + 1) * 64], PTn[:, cc, :], vw[:, cc, h * 64:(h + 1) * 64],
                                            start=(cc == 0), stop=(cc == 4)), r=[PTn, vw], w=["D1b"])
            _step(nxt_pre, 4)
        _drain(nxt_pre)
        if j + 1 < nown:
            load_win(j + 1)
        op('dve', lambda e: e.reciprocal(st2[:, 24:32], st2[:, 16:24]), r=["st2_rs%d" % h_ for h_ in range(8)], w=["st2_ri"])
        op('dve', lambda e: e.tensor_tensor(mixf[:, 512:1024].rearrange("p (s d) -> p s d", s=8),
                                            pOB.rearrange("p (s d) -> p s d", s=8),
                                            _bc(st2[:, 24:32], 2, [128, 8, 64]), ALU.mult), r=["D1b", "st2_ri"], w=[mixf])
        if dbg == 3:
            c.dma('sp', out[0:128, :], mixf[:], r=[mixf], out_final=True)
            c.finish()
            return nc
        for gi in range(2):
            op('dve', lambda e: e.scalar_tensor_tensor(s_sb[:, 0:512], mixf[:, gi * 512:(gi + 1) * 512], 1.0, mixf[:, gi * 512:(gi + 1) * 512],
                                                       ALU.mult, ALU.mult, accum_out=st2[:, 32 + gi:33 + gi]), r=[mixf], w=[st2, s_sb])
        op('dve', lambda e: e.tensor_scalar(st2[:, 34:36], st2[:, 32:34], 1.0 / 512, EPS, ALU.mult, ALU.add), r=[st2], w=[st2])
        op('act', lambda e: e.activation(st2[:, 38:40], st2[:, 34:36], AF.Ln), r=[st2], w=[st2])
        op('act', lambda e: e.activation(st2[:, 36:38], st2[:, 38:40], AF.Exp, scale=-0.5), r=[st2], w=[st2])
        for gi in range(2):
            op('act', lambda e: e.activation(mixn[:, gi * 512:(gi + 1) * 512], mixf[:, gi * 512:(gi + 1) * 512], AF.Identity,
                                             scale=st2[:, 36 + gi:37 + gi]), r=[mixf, st2], w=[mixn])
        for k in range(8):
            op('pe', lambda e: e.transpose(pTv[:, k, :], mixn[:, k * 128:(k + 1) * 128], identb[:]), r=[mixn, identb], w=["D0a"])
        op('dve', lambda e: e.tensor_tensor(mixT[:], pTv, _bc(gnT[:], 2, [128, 8, 128]), ALU.mult), r=["D0a", gnT], w=[mixT])
        for hh in range(2):
            for jj in range(8):
                op('pe', lambda e: e.matmul(D[1][:, hh * 512:(hh + 1) * 512], mixT[:, jj, :], Wout[:, jj, hh * 512:(hh + 1) * 512],
                                            start=(jj == 0), stop=(jj == 7)), r=[mixT, Wout], w=[Dk(1, hh)])
        xo = x1t[0]
        op('dve', lambda e: e.tensor_tensor(mixf[:], D[1][:, :], gate1_bc[:], ALU.mult), r=["D1a", "D1b", gate1_bc], w=[mixf])
        op('dve', lambda e: e.tensor_tensor(xo[:], mixf[:], xt[:], ALU.add), r=[mixf, xt], w=[xo])
        c.dma('sp', out[j * 128:(j + 1) * 128, :], xo[:], r=[xo], out_final=True)
    c.barrier()
    S1.close()
    if stop_after == 'B1':
        c.finish()
        return nc
    _phase_c(c, nc, nown, out, w_q, uvb, k1T, k2T, identb, neghalf, iota16, gate2_bc, A2_bc, B2_bc, gf_bc)
    c.finish()
    return nc


NB = 22
GS = 4
LEAD = 6


def _phase_c(c, nc, nown, out, w_q, uvb, k1T, k2T, identb, neghalf, iota16, gate2_bc, A2_bc, B2_bc, gf_bc, pipeline=True):
    op = c.op
    S2 = _Scope(c, "s2_")
    Wq = S2.sb("Wq", [128, 8, 2048], BF16)
    w_q_v = w_q.rearrange("(j p) n -> p j n", p=128)
    for k in range(2):
        op('pool', lambda e: e.dma_start(out=Wq[:, :, k * 1024:(k + 1) * 1024], in_=w_q_v[:, :, k * 1024:(k + 1) * 1024]),
           w=[Wq], dma=True)
    x1b = [S2.sb("x1b%d" % i, [128, 1024], F32) for i in range(2)]
    h2s = [S2.sb("h2_%d" % i, [128, 1024], F32) for i in range(1)]
    h2bs = [S2.sb("h2b%d" % i, [128, 1024], BF16) for i in range(2)]
    prod = [S2.sb("prod%d" % i, [128, 1024], BF16) for i in range(4)]
    eidxs = [S2.sb("eidx%d" % i, [128, 128], I32) for i in range(2)]
    gws = [S2.sb("gw%d" % i, [128, 8, 16], F32) for i in range(2)]
    st = S2.sb("st", [128, 16], F32)
    stt = S2.sb("stt", [128, 16], F32)
    xn2 = S2.sb("xn2", [128, 1024], F32)
    h2T = S2.sb("h2T", [128, 8, 128], BF16)
    qT = S2.sb("qT", [128, 16, 128], BF16)
    s_sb = S2.sb("s_sb", [128, 16, 128], F32)
    wk = S2.sb("wk", [128, 256], F32)
    vals = S2.sb("vals", [128, 16, 16], F32)
    idxs = S2.sb("idxs", [128, 16, 16], U32)
    idxf = S2.sb("idxf", [128, 16, 16], F32)
    cand = S2.sb("cand", [128, 8, 256], F32)
    eq = cand
    eqv = cand[:, :, :].rearrange("p h (k m) -> p h k m", k=16)
    tv = S2.sb("tv", [128, 8, 16], F32)
    tp = S2.sb("tp", [128, 8, 16], U32)
    ij = S2.sb("ij", [128, 2, 128], U32)
    ijf = S2.sb("ijf", [128, 2, 128], F32)
    I12 = S2.sb("I12", [128, 2, 128], F32)
    eidf = S2.sb("eidf", [128, 128], F32)
    sm = S2.sb("sm", [128, 16], F32)
    a_t = S2.sb("a_t", [128, 128], F32)
    ga = S2.sb("ga", [128, 128], F32)
    wgt = S2.sb("wgt", [128, 128], F32)
    gb = [S2.sb("gb%d" % i, [128, 2048], BF16) for i in range(NB)]
    dg = [S2.sb("dg%d" % i, [128, 128], BF16) for i in range(4)]
    yt = h2s[0]
    xo = xn2
    D = [S2.ps("D%d" % i, [128, 1024], F32) for i in range(4)]
    pTv = D[0][:, 0:512].bitcast(BF16).rearrange("p (j n) -> p j n", j=8)
    pacc = D[3]

    def load(j):
        c.dma('sp', x1b[j % 2][:], out[j * 128:(j + 1) * 128, :], w=[x1b[j % 2]])

    def front(j):
        x1 = x1b[j % 2]; h2 = h2s[0]; h2b = h2bs[j % 2]; eidx = eidxs[j % 2]; gw = gws[j % 2]
        op('act', lambda e: e.activation(xn2[:], x1[:], AF.Square, accum_out=st[:, 0:1]), r=[x1], w=[st, xn2]); yield
        op('dve', lambda e: e.tensor_scalar(st[:, 1:2], st[:, 0:1], 1.0 / D_MODEL, EPS, ALU.mult, ALU.add), r=[st], w=[st]); yield
        op('act', lambda e: e.activation(st[:, 3:4], st[:, 1:2], AF.Ln), r=[st], w=[st])
        op('act', lambda e: e.activation(st[:, 2:3], st[:, 3:4], AF.Exp, scale=-0.5), r=[st], w=[st])
        op('act', lambda e: e.activation(xn2[:], x1[:], AF.Identity, scale=st[:, 2:3]), r=[x1, st], w=[xn2]); yield
        op('dve', lambda e: e.tensor_tensor(h2[:], xn2[:], A2_bc[:], ALU.mult), r=[xn2, A2_bc], w=[h2]); yield
        op('dve', lambda e: e.tensor_tensor(h2[:], h2[:], B2_bc[:], ALU.add), r=[h2, B2_bc], w=[h2]); yield
        op('act', lambda e: e.copy(h2b[:], h2[:]), r=[h2], w=[h2b])
        for k in range(8):
            op('pe', lambda e: e.transpose(pTv[:, k, :], h2b[:, k * 128:(k + 1) * 128], identb[:]), r=[h2b, identb], w=["E0a"])
        op('dve', lambda e: e.tensor_copy(h2T[:], pTv), r=["E0a"], w=[h2T]); yield
        for hf in range(2):
            dq = D[2][:, :].rearrange("p (s n) -> p s n", s=8)
            for f8 in range(8):
                f = hf * 8 + f8
                for jj in range(8):
                    op('pe', lambda e: e.matmul(dq[:, f8, :], Wq[:, jj, f * 128:(f + 1) * 128], h2T[:, jj, :],
                                                start=(jj == 0), stop=(jj == 7)), r=[Wq, h2T], w=["E2"])
            op('act', lambda e: e.copy(qT[:, hf * 8:(hf + 1) * 8, :], dq), r=["E2"], w=[qT]); yield
        for hf in range(2):
            ds = D[1][:, :].rearrange("p (s n) -> p s n", s=8)
            for f8 in range(8):
                f = hf * 8 + f8
                kk = k1T if f % 2 == 0 else k2T
                op('pe', lambda e: e.matmul(ds[:, f8, :], qT[:, f, :], kk[:], start=True, stop=True), r=[qT, kk], w=["E1"])
            op('act', lambda e: e.copy(s_sb[:, hf * 8:(hf + 1) * 8, :], ds), r=["E1"], w=[s_sb]); yield
        for f in range(16):
            op('dve', lambda e: e.max(vals[:, f, 0:8], s_sb[:, f, :]), r=[s_sb], w=[vals]); yield
            op('dve', lambda e: e.max_index(idxs[:, f, 0:8], vals[:, f, 0:8], s_sb[:, f, :]), r=[s_sb, vals], w=[idxs]); yield
            op('dve', lambda e: e.match_replace(wk[:, 0:128], vals[:, f, 0:8], s_sb[:, f, :], -1e30), r=[s_sb, vals], w=[wk]); yield
            op('dve', lambda e: e.max(vals[:, f, 8:16], wk[:, 0:128]), r=[wk], w=[vals]); yield
            op('dve', lambda e: e.max_index(idxs[:, f, 8:16], vals[:, f, 8:16], wk[:, 0:128]), r=[wk, vals], w=[idxs]); yield
        v4 = vals[:, :, :].rearrange("p (h a) k -> p h a k", a=2)
        op('dve', lambda e: e.tensor_tensor(cand[:, :, :].rearrange("p h (i k) -> p h i k", i=16),
                                            _bc(v4[:, :, 0, :], 3, [128, 8, 16, 16]), _bc(v4[:, :, 1, :], 2, [128, 8, 16, 16]), ALU.add),
           r=[vals], w=[cand]); yield
        for h in range(8):
            op('dve', lambda e: e.max(tv[:, h, 0:8], cand[:, h, :]), r=[cand], w=[tv]); yield
            op('dve', lambda e: e.max_index(tp[:, h, 0:8], tv[:, h, 0:8], cand[:, h, :]), r=[cand, tv], w=[tp]); yield
            op('dve', lambda e: e.match_replace(wk[:], tv[:, h, 0:8], cand[:, h, :], -1e30), r=[cand, tv], w=[wk]); yield
            op('dve', lambda e: e.max(tv[:, h, 8:16], wk[:]), r=[wk], w=[tv]); yield
            op('dve', lambda e: e.max_index(tp[:, h, 8:16], tv[:, h, 8:16], wk[:]), r=[wk, tv], w=[tp]); yield
        tpf = tp[:, :, :].rearrange("p h k -> p (h k)")
        op('dve', lambda e: e.tensor_single_scalar(ij[:, 0, :], tpf, 4, ALU.logical_shift_right), r=[tp], w=[ij]); yield
        op('dve', lambda e: e.tensor_single_scalar(ij[:, 1, :], tpf, 15, ALU.bitwise_and), r=[tp, ij], w=[ij]); yield
        op('dve', lambda e: e.tensor_copy(ijf[:], ij[:]), r=[ij], w=[ijf]); yield
        op('dve', lambda e: e.tensor_copy(idxf[:], idxs[:]), r=[idxs], w=[idxf]); yield
        i4 = idxf[:, :, :].rearrange("p (h a) k -> p h a k", a=2)
        iot = iota16[:, :].unsqueeze(1).unsqueeze(1).to_broadcast([128, 8, 16, 16])
        for a in range(2):
            sel = ijf[:, a, :].rearrange("p (h k) -> p h k", h=8)
            op('dve', lambda e: e.tensor_tensor(eqv, _bc(sel, 3, [128, 8, 16, 16]), iot, ALU.is_equal), r=[ijf, iota16], w=[eq]); yield
            op('dve', lambda e: e.tensor_tensor(eqv, eqv, _bc(i4[:, :, a, :], 2, [128, 8, 16, 16]), ALU.mult), r=[eq, idxf], w=[eq]); yield
            op('dve', lambda e: e.tensor_reduce(I12[:, a, :].rearrange("p (h k) -> p h k", h=8), eqv, AX.X, ALU.add), r=[eq], w=[I12]); yield
        op('dve', lambda e: e.scalar_tensor_tensor(eidf[:], I12[:, 0, :], 128.0, I12[:, 1, :], ALU.mult, ALU.add), r=[I12], w=[eidf]); yield
        op('dve', lambda e: e.tensor_copy(eidx[:], eidf[:]), r=[eidf], w=[eidx]); yield
        op('dve', lambda e: e.tensor_tensor(gw[:], tv[:], _bc(tv[:, :, 0], 2, [128, 8, 16]), ALU.subtract), r=[tv], w=[gw]); yield
        op('act', lambda e: e.activation(gw[:], gw[:], AF.Exp), r=[gw], w=[gw])
        op('dve', lambda e: e.tensor_reduce(sm[:, 0:8], gw[:], AX.X, ALU.add), r=[gw], w=[sm]); yield
        op('dve', lambda e: e.reciprocal(sm[:, 8:16], sm[:, 0:8]), r=[sm], w=[sm]); yield
        op('dve', lambda e: e.tensor_tensor(gw[:], gw[:], _bc(sm[:, 8:16], 2, [128, 8, 16]), ALU.mult), r=[gw, sm], w=[gw]); yield

    def drain(g):
        if g is not None:
            for _ in g:
                pass

    def step(g, n=1):
        if g is not None:
            for _ in range(n):
                try:
                    next(g)
                except StopIteration:
                    return

    if nown > 0:
        load(0)
        drain(front(0))
    for j in range(nown):
        nxt = None
        if j + 1 < nown:
            load(j + 1)
            nxt = front(j + 1)
            if not pipeline:
                drain(nxt); nxt = None
        x1 = x1b[j % 2]; h2b = h2bs[j % 2]; eidx = eidxs[j % 2]; gw = gws[j % 2]
        gwf = gw[:, :, :].rearrange("p h k -> p (h k)")

        def gather(s, jb=j):
            b = gb[s % NB]
            ei = eidxs[jb % 2]
            op('pool', lambda e: e.indirect_dma_start(out=b[:], out_offset=None, in_=uvb,
                                                      in_offset=bass.IndirectOffsetOnAxis(ap=ei[:, s:s + 1], axis=0)),
               r=[ei], w=[b], dma=True)
        def finish(g0):
            gs_ = slice(g0, g0 + GS)
            op('dve', lambda e: e.tensor_tensor(wgt[:, gs_], ga[:, gs_], gwf[:, gs_], ALU.mult), r=[ga, gw], w=[wgt])
            for s in range(g0, g0 + GS):
                b = gb[s % NB]; d = dg[s % 4]
                if True:
                    op('act', lambda e: e.activation(d[:], identb[:], AF.Identity, scale=wgt[:, s:s + 1]), r=[identb, wgt], w=[d])
                else:
                    op('dve', lambda e: e.tensor_scalar(d[:], identb[:], wgt[:, s:s + 1], None, ALU.mult), r=[identb, wgt], w=[d])
                for hf in range(2):
                    op('pe', lambda e: e.matmul(pacc[:, hf * 512:(hf + 1) * 512], d[:], b[:, 1024 + hf * 512:1024 + (hf + 1) * 512],
                                                start=(s == 0), stop=(s == 127)), r=[d, b], w=["E3"])
        if j == 0:
            for s in range(LEAD):
                gather(s)
        prev = None
        for g0 in range(0, 128, GS):
            for s in range(g0, g0 + GS):
                if s + LEAD < 128:
                    gather(s + LEAD)
                b = gb[s % NB]
                pr_ = prod[s % 4]
                if s % GS == GS - 1:
                    op('dve', lambda e: e.scalar_tensor_tensor(pr_[:], b[:, 0:1024], 1.0, h2b[:], ALU.mult, ALU.mult,
                                                               accum_out=a_t[:, s:s + 1]), r=[b, h2b], w=[pr_, "a_t_dve"])
                else:
                    op('dve', lambda e: e.tensor_tensor(pr_[:], b[:, 0:1024], h2b[:], ALU.mult), r=[b, h2b], w=[pr_])
                    op('act', lambda e: e.activation(pr_[:], pr_[:], AF.Identity, accum_out=a_t[:, s:s + 1]),
                       r=[pr_], w=([pr_, "a_t_act"] if s % GS == GS - 2 else [pr_]))
                step(nxt, 1 + (s % 2))
            gs_ = slice(g0, g0 + GS)
            op('act', lambda e: e.activation(ga[:, gs_], a_t[:, gs_], AF.Gelu), r=["a_t_act", "a_t_dve"], w=[ga])
            if prev is not None:
                finish(prev)
            prev = g0
        finish(prev)
        drain(nxt)
        if j + 1 < nown:
            for s in range(LEAD):
                gather(s, j + 1)
        op('dve', lambda e: e.tensor_tensor(yt[:], pacc[:, :], gate2_bc[:], ALU.mult), r=["E3", gate2_bc], w=[yt])
        op('dve', lambda e: e.tensor_tensor(yt[:], yt[:], x1[:], ALU.add), r=[yt, x1], w=[yt])
        op('act', lambda e: e.activation(xo[:], yt[:], AF.Square, accum_out=stt[:, 4:5]), r=[yt], w=[stt, xo])
        op('dve', lambda e: e.tensor_scalar(stt[:, 5:6], stt[:, 4:5], 1.0 / D_MODEL, EPS, ALU.mult, ALU.add), r=[stt], w=[stt])
        op('act', lambda e: e.activation(stt[:, 7:8], stt[:, 5:6], AF.Ln), r=[stt], w=[stt])
        op('act', lambda e: e.activation(stt[:, 6:7], stt[:, 7:8], AF.Exp, scale=-0.5), r=[stt], w=[stt])
        op('act', lambda e: e.activation(xo[:], yt[:], AF.Identity, scale=stt[:, 6:7]), r=[yt, stt], w=[xo])
        op('dve', lambda e: e.tensor_tensor(yt[:], xo[:], gf_bc[:], ALU.mult), r=[xo, gf_bc], w=[yt])
        c.dma('sp', out[j * 128:(j + 1) * 128, :], yt[:], r=[yt], out_final=True)
    c.barrier()
    S2.close()


_ROPE_CACHE = {}


def _rope_tables(half):
    if half in _ROPE_CACHE:
        return _ROPE_CACHE[half]
    t = np.arange(8192)
    rho = t // 64
    col = t % 64
    row = rho if half == 0 else 127 - rho
    freqs = (np.float32(10000.0) ** (-np.arange(16, dtype=np.float32) / np.float32(16))).astype(np.float32)
    ar = row.astype(np.float32)[:, None] * freqs[None, :]
    ac = col.astype(np.float32)[:, None] * freqs[None, :]
    cr, sr, cc, sc = np.cos(ar), np.sin(ar), np.cos(ac), np.sin(ac)
    cos64 = np.concatenate([cr, cr, cc, cc], axis=1)
    sin64 = np.concatenate([-sr, sr, -sc, sc], axis=1)
    tab = np.stack([cos64, sin64], axis=1).astype(np.float32).reshape(64, 128, 2, 64)
    _ROPE_CACHE[half] = np.ascontiguousarray(tab)
    return _ROPE_CACHE[half]


def _nbias_tables(rpb, half):
    out = np.full((3, 128, 8, 640), NEG, dtype=np.float32)
    p = np.arange(128)
    a, cq = p // 64, p % 64
    kk = np.arange(640)
    kro, ck = kk // 64, kk % 64
    for j in range(3):
        w0 = min(max(2 * j - 4, 0), 58)
        rho = 2 * j + a
        kap = w0 + kro
        if half == 0:
            r, kr = rho, kap
        else:
            r, kr = 127 - rho, 127 - kap
        rs = np.clip(r - 4, 0, 120)
        cs = np.clip(cq - 8, 0, 48)
        vr = (kr[None, :] >= rs[:, None]) & (kr[None, :] <= rs[:, None] + 7)
        vc = (ck[None, :] >= cs[:, None]) & (ck[None, :] <= cs[:, None] + 15)
        valid = vr & vc
        dri = np.clip(kr[None, :] - r[:, None] + 7, 0, 14)
        dci = np.clip(ck[None, :] - cq[:, None] + 15, 0, 30)
        g = rpb[:, dri, dci]
        g = np.transpose(g, (1, 0, 2))
        out[j] = np.where(valid[:, None, :], g, np.float32(NEG))
    return out


_QPERM = np.array([(4 * g + i) * 64 + d for i in range(4) for g in range(2) for d in range(64)])


def _fm(v):
    return np.ascontiguousarray(np.asarray(v, dtype=np.float32).reshape(8, 128).T)


def make_in_maps(inputs):
    f = lambda k: np.asarray(inputs[k], dtype=np.float32)
    x = f("x"); cc = f("c")
    w_in = f("w_in")[0].copy()
    w_in[:, :512] = w_in[:, _QPERM]
    gn = np.concatenate([f("group_norm_a_g")[0], f("group_norm_b_g")[0]])
    shared = {
        "w_ada": np.ascontiguousarray(f("w_ada")[0]), "b_ada": np.ascontiguousarray(f("b_ada")[0][None, :]),
        "g1T": _fm(f("norm1_g")[0]), "g2row": np.ascontiguousarray(f("norm2_g")[0][None, :]),
        "gfrow": np.ascontiguousarray(f("final_norm_g")[None, :]), "w_in": np.ascontiguousarray(w_in),
        "qg": np.ascontiguousarray(f("q_norm_g")[0][None, :]), "kg": np.ascontiguousarray(f("k_norm_g")[0][None, :]),
        "gnT": _fm(gn), "w_out": np.ascontiguousarray(f("w_out")[0]), "w_q": np.ascontiguousarray(f("peer_w_query")[0]),
        "k1T": np.ascontiguousarray(f("peer_sub_keys_1")[0].T), "k2T": np.ascontiguousarray(f("peer_sub_keys_2")[0].T),
        "peer_uv": np.ascontiguousarray(np.stack([f("peer_u")[0], f("peer_v")[0]], axis=1).reshape(16384, 2048)),
    }
    rpb = f("natten_rpb")[0]
    nb = [_nbias_tables(rpb, h) for h in range(2)]
    maps = []
    for core in range(8):
        b, half = core // 2, core % 2
        xb = x[b]
        if half == 1:
            xb = xb.reshape(128, 64, 1024)[::-1].reshape(8192, 1024)
        m = dict(shared)
        m["xs"] = np.ascontiguousarray(xb)
        m["cT"] = _fm(cc[b])
        m["rope"] = _rope_tables(half)
        m["nbias"] = nb[half]
        maps.append(m)
    return maps


def assemble(results):
    out = np.empty((4, 8192, 1024), dtype=np.float32)
    for core in range(8):
        b, half = core // 2, core % 2
        o = np.asarray(results[core]["out"], dtype=np.float32).reshape(64, 64, 1024)
        if half == 0:
            out[b, :4096] = o.reshape(4096, 1024)
        else:
            out[b].reshape(128, 64, 1024)[64:] = o[::-1]
    return out


_NC_CACHE = {}


def kernel(**inputs):
    if "nc" not in _NC_CACHE:
        _NC_CACHE["nc"] = build_nc()
    nc = _NC_CACHE["nc"]
    maps = make_in_maps(inputs)
    res = run_bass_kernel_spmd(nc, maps, core_ids=list(range(8)))
    return assemble(res.results)
```
